# Optimizing a Trainium2 kernel written in Bass

```python
import math
import jax, jax.numpy as jnp
from jax import lax
import numpy as np

D_MODEL = 1024
BATCH = 8
SEQ = 4096
DEPTH = 1
DEC_BATCH = 128
DEC_SEQ = 1
PAST_LEN = 8192
PAGE_SIZE = 128

HEAD_DIM = 64
HEADS_PER_GROUP = 4
DILATION_GROUPS = ((128, 1), (512, 4), (2048, 16))
N_GROUPS = len(DILATION_GROUPS)
N_ATTN_HEADS = HEADS_PER_GROUP * N_GROUPS
ATTN_WIDTH = N_ATTN_HEADS * HEAD_DIM
ATTN_OUT = HEADS_PER_GROUP * HEAD_DIM
ROPE_THETA = 10000.0
SSM_WIDTH = D_MODEL // 4
SSM_GROUP = 16
SSM_GROUPS = SSM_WIDTH // SSM_GROUP
SSM_STATE = 64
IN_WIDTH = 3 * ATTN_WIDTH + SSM_WIDTH
N_EXPERTS = 64
TOP_K = 8
EXPERT_FF = D_MODEL // 4
SHARED_FF = EXPERT_FF
ROUTED_SCALE = 2.5
MOE_BLOCK = 128
PLE_DIM = 256
DN_ALPHA = (2.0 * DEPTH) ** 0.25
DN_BETA = (8.0 * DEPTH) ** -0.25
LN_EPS = 1e-5

kernel_name = 'hybrid_s5_dilated_attn_moe_step'


def layer_norm(x, g, b):
    xf = x.astype(jnp.float32)
    mu = jnp.mean(xf, axis=-1, keepdims=True)
    var = jnp.mean(jnp.square(xf - mu), axis=-1, keepdims=True)
    return ((xf - mu) * lax.rsqrt(var + LN_EPS) * g + b).astype(x.dtype)


def rope(x, pos):
    half = HEAD_DIM // 2
    inv = ROPE_THETA ** (-jnp.arange(half, dtype=jnp.float32) / half)
    ang = pos.astype(jnp.float32)[:, None] * inv[None, :]
    cos = jnp.cos(ang)[None, :, None, :]
    sin = jnp.sin(ang)[None, :, None, :]
    xf = x.astype(jnp.float32)
    x1, x2 = xf[..., :half], xf[..., half:]
    return jnp.concatenate([x1 * cos - x2 * sin, x2 * cos + x1 * sin], axis=-1).astype(x.dtype)


def dilated_attn_prompt(q, k, v, window, dilation):
    B, S, H, Dh = q.shape
    nback = window // dilation
    span = nback * dilation
    s_pad = -(-S // span) * span
    nb = s_pad // span
    padw = ((0, 0), (0, s_pad - S), (0, 0), (0, 0))

    def blocks(t):
        return jnp.pad(t, padw).reshape(B, nb, nback, dilation, H, Dh)

    def with_prev(t):
        prev = jnp.concatenate([jnp.zeros_like(t[:, :1]), t[:, :-1]], axis=1)
        return jnp.concatenate([prev, t], axis=2)

    qb = blocks(q)
    kk = with_prev(blocks(k))
    vv = with_prev(blocks(v))
    logits = jnp.einsum('bcqrhd,bckrhd->bcrhqk', qb, kk, preferred_element_type=jnp.float32) * (Dh ** -0.5)
    qi = jnp.arange(nback)[:, None]
    kj = jnp.arange(2 * nback)[None, :]
    dist = qi + nback - kj
    band = (dist >= 0) & (dist <= nback)
    first = jnp.arange(nb)[:, None, None] > 0
    mask = band[None] & (first | (kj >= nback)[None])
    logits = jnp.where(mask[None, :, None, None], logits, -jnp.inf)
    mx = jnp.max(logits, axis=-1)
    p = jnp.exp(logits - mx[..., None])
    den = jnp.sum(p, axis=-1)
    num = jnp.einsum('bcrhqk,bckrhd->bcqrhd', p, vv.astype(jnp.float32))
    num = num.reshape(B, s_pad, H, Dh)[:, :S]
    den = jnp.transpose(den, (0, 1, 4, 2, 3)).reshape(B, s_pad, H)[:, :S]
    mx = jnp.transpose(mx, (0, 1, 4, 2, 3)).reshape(B, s_pad, H)[:, :S]
    return num, den, mx


def dilated_attn_sample(q, k_all, v_all, window, dilation):
    B, T, H, Dh = q.shape
    L = k_all.shape[1] - T
    nback = window // dilation
    idx = L + jnp.arange(T)[:, None] - dilation * jnp.arange(nback + 1)[None, :]
    valid = idx >= 0
    idx_c = jnp.maximum(idx, 0)
    kg = k_all[:, idx_c]
    vg = v_all[:, idx_c]
    logits = jnp.einsum('bthd,btjhd->bthj', q, kg, preferred_element_type=jnp.float32) * (Dh ** -0.5)
    logits = jnp.where(valid[None, :, None, :], logits, -jnp.inf)
    mx = jnp.max(logits, axis=-1)
    p = jnp.exp(logits - mx[..., None])
    den = jnp.sum(p, axis=-1)
    num = jnp.einsum('bthj,btjhd->bthd', p, vg.astype(jnp.float32))
    return num, den, mx


def merge_dilations(parts):
    m = parts[0][2]
    for _, _, mx in parts[1:]:
        m = jnp.maximum(m, mx)
    num = 0.0
    den = 0.0
    for n_g, d_g, mx in parts:
        scale = jnp.exp(mx - m)
        num = num + n_g * scale[..., None]
        den = den + d_g * scale
    o = num / den[..., None]
    B, T = o.shape[0], o.shape[1]
    return o.reshape(B, T, ATTN_OUT)


def attend_prompt(q, k, v):
    parts, kv_rows = [], []
    S = q.shape[1]
    for g, (win, dil) in enumerate(DILATION_GROUPS):
        sl = slice(g * HEADS_PER_GROUP, (g + 1) * HEADS_PER_GROUP)
        parts.append(dilated_attn_prompt(q[:, :, sl], k[:, :, sl], v[:, :, sl], win, dil))
        keep = min(win, S)
        kv_rows.append(jnp.stack([k[:, S - keep:, sl], v[:, S - keep:, sl]], axis=2))
    return merge_dilations(parts), kv_rows


def make_attend_sample(layer_caches):
    def attend(q, k, v):
        parts, kv_rows = [], []
        for g, (win, dil) in enumerate(DILATION_GROUPS):
            sl = slice(g * HEADS_PER_GROUP, (g + 1) * HEADS_PER_GROUP)
            kv_new = jnp.stack([k[:, :, sl], v[:, :, sl]], axis=2)
            kv_all = jnp.concatenate([layer_caches[g].astype(kv_new.dtype), kv_new], axis=1)
            parts.append(dilated_attn_sample(q[:, :, sl], kv_all[:, :, 0], kv_all[:, :, 1], win, dil))
            kv_rows.append(kv_new)
        return merge_dilations(parts), kv_rows
    return attend


def s5_scan(u, h0, w):
    B, T, _ = u.shape
    f32 = jnp.float32
    uf = u.astype(f32).reshape(B, T, SSM_GROUPS, SSM_GROUP)
    dt = jnp.exp(w['log_dt'].astype(f32))[:, None]
    ar = w['a_re'].astype(f32)
    ai = w['a_im'].astype(f32)
    mag = jnp.exp(ar * dt)
    abar_re = mag * jnp.cos(ai * dt)
    abar_im = mag * jnp.sin(ai * dt)
    a2 = ar * ar + ai * ai
    nr = abar_re - 1.0
    coef_re = (nr * ar + abar_im * ai) / a2
    coef_im = (abar_im * ar - nr * ai) / a2
    b_re = w['b_re'].astype(f32)
    b_im = w['b_im'].astype(f32)
    bb_re = coef_re[..., None] * b_re - coef_im[..., None] * b_im
    bb_im = coef_re[..., None] * b_im + coef_im[..., None] * b_re
    bu_re = jnp.einsum('btgc,gpc->btgp', uf, bb_re)
    bu_im = jnp.einsum('btgc,gpc->btgp', uf, bb_im)
    h0_re = h0[..., 0].astype(f32)
    h0_im = h0[..., 1].astype(f32)
    bu_re = bu_re.at[:, 0].add(abar_re * h0_re - abar_im * h0_im)
    bu_im = bu_im.at[:, 0].add(abar_re * h0_im + abar_im * h0_re)
    a_t_re = jnp.broadcast_to(abar_re, bu_re.shape)
    a_t_im = jnp.broadcast_to(abar_im, bu_im.shape)

    def combine(e1, e2):
        a1r, a1i, b1r, b1i = e1
        a2r, a2i, b2r, b2i = e2
        return (a1r * a2r - a1i * a2i, a1r * a2i + a1i * a2r,
                a2r * b1r - a2i * b1i + b2r, a2r * b1i + a2i * b1r + b2i)

    _, _, h_re, h_im = lax.associative_scan(combine, (a_t_re, a_t_im, bu_re, bu_im), axis=1)
    y = (jnp.einsum('btgp,gcp->btgc', h_re, w['c_re'].astype(f32))
         - jnp.einsum('btgp,gcp->btgc', h_im, w['c_im'].astype(f32)))
    y = y.reshape(B, T, SSM_WIDTH) + w['d_skip'].astype(f32) * uf.reshape(B, T, SSM_WIDTH)
    h_last = jnp.stack([h_re[:, -1], h_im[:, -1]], axis=-1)
    return y, h_last


def moe(x, w):
    B, T, D = x.shape
    n = B * T
    xf = x.reshape(n, D)
    scores = jax.nn.sigmoid(jnp.einsum('nd,de->ne', xf, w['w_router'], preferred_element_type=jnp.float32))
    _, top_idx = lax.top_k(scores + w['router_bias'].astype(jnp.float32), TOP_K)
    top_s = jnp.take_along_axis(scores, top_idx, axis=-1)
    gates = top_s / jnp.sum(top_s, axis=-1, keepdims=True) * ROUTED_SCALE
    flat_e = top_idx.reshape(-1)
    flat_g = gates.reshape(-1)
    order = jnp.argsort(flat_e)
    se = flat_e[order]
    stok = (order // TOP_K).astype(jnp.int32)
    sg = flat_g[order]
    counts = jnp.zeros((N_EXPERTS,), jnp.int32).at[flat_e].add(1)
    start = jnp.cumsum(counts) - counts
    pcounts = (counts + MOE_BLOCK - 1) // MOE_BLOCK * MOE_BLOCK
    pend = jnp.cumsum(pcounts)
    pstart = pend - pcounts
    dest = pstart[se] + (jnp.arange(n * TOP_K, dtype=jnp.int32) - start[se])
    n_blocks = -(-(n * TOP_K) // MOE_BLOCK) + N_EXPERTS
    rows = n_blocks * MOE_BLOCK
    row_tok = jnp.full((rows,), n, jnp.int32).at[dest].set(stok)
    row_gate = jnp.zeros((rows,), jnp.float32).at[dest].set(sg)
    block_exp = jnp.minimum(jnp.searchsorted(pend, jnp.arange(n_blocks, dtype=jnp.int32) * MOE_BLOCK, side='right'),
                            N_EXPERTS - 1)
    x_pad = jnp.concatenate([xf, jnp.zeros((1, D), xf.dtype)], axis=0)
    w1, w3, w2 = w['w1'], w['w3'], w['w2']

    def body(b, acc):
        tok = lax.dynamic_slice(row_tok, (b * MOE_BLOCK,), (MOE_BLOCK,))
        g = lax.dynamic_slice(row_gate, (b * MOE_BLOCK,), (MOE_BLOCK,))
        e = block_exp[b]
        xb = x_pad[tok]
        h = jax.nn.silu(xb @ w1[e]) * (xb @ w3[e])
        yb = (h @ w2[e]).astype(jnp.float32)
        return acc.at[tok].add(yb * g[:, None])

    acc = lax.fori_loop(0, n_blocks, body, jnp.zeros((n + 1, D), jnp.float32))
    shared = (jax.nn.silu(xf @ w['ws1']) * (xf @ w['ws3'])) @ w['ws2']
    return (acc[:n] + shared).reshape(B, T, D).astype(x.dtype)


def trunk_layer(x, p_l, pos, attend, h0, w):
    B, T, _ = x.shape
    proj = x @ w['w_in']
    a = ATTN_WIDTH
    q = rope(proj[..., :a].reshape(B, T, N_ATTN_HEADS, HEAD_DIM), pos)
    k = rope(proj[..., a:2 * a].reshape(B, T, N_ATTN_HEADS, HEAD_DIM), pos)
    v = proj[..., 2 * a:3 * a].reshape(B, T, N_ATTN_HEADS, HEAD_DIM)
    u = proj[..., 3 * a:]
    attn_o, kv_rows = attend(q, k, v)
    ssm_y, h_last = s5_scan(u, h0, w)
    s = jax.nn.gelu(ssm_y)
    s = s * jax.nn.sigmoid(s @ w['w_glu'] + w['b_glu'])
    gates = jax.nn.sigmoid(x @ w['w_gate'] + w['b_gate'])
    merged = (gates[..., :D_MODEL] * (attn_o @ w['w_attn_br'])
              + gates[..., D_MODEL:] * (s @ w['w_ssm_br']))
    mix = (merged @ w['w_out']).astype(x.dtype)
    x = layer_norm(DN_ALPHA * x + mix, w['ln1_g'], w['ln1_b'])
    ff = moe(x, w)
    ple = (jax.nn.sigmoid(x @ w['w_ple_gate']) * (p_l @ w['w_ple'])).astype(x.dtype)
    x = layer_norm(DN_ALPHA * x + ff + ple, w['ln2_g'], w['ln2_b'])
    return x, kv_rows, h_last


def setup_inputs(seed: int = 0) -> dict:
    key = jax.random.key(seed)
    ks = iter(jax.random.split(key, 48))
    f32 = jnp.float32

    def nrm(shape, scale):
        return jax.random.normal(next(ks), shape, f32) * scale

    cache_shape = lambda win: (DEPTH, DEC_BATCH, min(win, PAST_LEN), 2, HEADS_PER_GROUP, HEAD_DIM)
    inp = {}
    inp['x_prompt'] = nrm((BATCH, SEQ, D_MODEL), 1.0)
    inp['x_sample'] = nrm((DEC_BATCH, DEC_SEQ, D_MODEL), 1.0)
    inp['cache_kv_w128'] = nrm(cache_shape(128), 1.0)
    inp['cache_kv_w512'] = nrm(cache_shape(512), 1.0)
    inp['cache_kv_w2048'] = nrm(cache_shape(2048), 1.0)
    inp['state_ssm'] = nrm((DEPTH, DEC_BATCH, SSM_GROUPS, SSM_STATE, 2), 0.5)
    inp['p_prompt'] = nrm((DEPTH, BATCH, SEQ, PLE_DIM), 1.0)
    inp['p_sample'] = nrm((DEPTH, DEC_BATCH, DEC_SEQ, PLE_DIM), 1.0)
    inp['w_in'] = nrm((DEPTH, D_MODEL, IN_WIDTH), D_MODEL ** -0.5)
    inp['a_re'] = -0.5 + nrm((DEPTH, SSM_GROUPS, SSM_STATE), 0.01)
    inp['a_im'] = math.pi * jnp.arange(SSM_STATE, dtype=f32) + nrm((DEPTH, SSM_GROUPS, SSM_STATE), 0.01)
    inp['log_dt'] = jax.random.uniform(next(ks), (DEPTH, SSM_GROUPS), f32, math.log(1e-3), math.log(1e-1))
    inp['b_re'] = nrm((DEPTH, SSM_GROUPS, SSM_STATE, SSM_GROUP), (2.0 * SSM_GROUP) ** -0.5)
    inp['b_im'] = nrm((DEPTH, SSM_GROUPS, SSM_STATE, SSM_GROUP), (2.0 * SSM_GROUP) ** -0.5)
    inp['c_re'] = nrm((DEPTH, SSM_GROUPS, SSM_GROUP, SSM_STATE), (2.0 * SSM_STATE) ** -0.5)
    inp['c_im'] = nrm((DEPTH, SSM_GROUPS, SSM_GROUP, SSM_STATE), (2.0 * SSM_STATE) ** -0.5)
    inp['d_skip'] = nrm((DEPTH, SSM_WIDTH), 1.0)
    inp['w_glu'] = nrm((DEPTH, SSM_WIDTH, SSM_WIDTH), SSM_WIDTH ** -0.5)
    inp['b_glu'] = nrm((DEPTH, SSM_WIDTH), 0.02)
    inp['w_attn_br'] = nrm((DEPTH, ATTN_OUT, D_MODEL), ATTN_OUT ** -0.5)
    inp['w_ssm_br'] = nrm((DEPTH, SSM_WIDTH, D_MODEL), SSM_WIDTH ** -0.5)
    inp['w_gate'] = nrm((DEPTH, D_MODEL, 2 * D_MODEL), D_MODEL ** -0.5)
    inp['b_gate'] = nrm((DEPTH, 2 * D_MODEL), 0.02)
    inp['w_out'] = nrm((DEPTH, D_MODEL, D_MODEL), D_MODEL ** -0.5 * DN_BETA)
    inp['ln1_g'] = 1.0 + nrm((DEPTH, D_MODEL), 0.01)
    inp['ln1_b'] = nrm((DEPTH, D_MODEL), 0.01)
    inp['w_router'] = nrm((DEPTH, D_MODEL, N_EXPERTS), D_MODEL ** -0.5)
    inp['router_bias'] = nrm((DEPTH, N_EXPERTS), 0.01)
    inp['w1'] = nrm((DEPTH, N_EXPERTS, D_MODEL, EXPERT_FF), D_MODEL ** -0.5)
    inp['w3'] = nrm((DEPTH, N_EXPERTS, D_MODEL, EXPERT_FF), D_MODEL ** -0.5)
    inp['w2'] = nrm((DEPTH, N_EXPERTS, EXPERT_FF, D_MODEL), EXPERT_FF ** -0.5 * DN_BETA)
    inp['ws1'] = nrm((DEPTH, D_MODEL, SHARED_FF), D_MODEL ** -0.5)
    inp['ws3'] = nrm((DEPTH, D_MODEL, SHARED_FF), D_MODEL ** -0.5)
    inp['ws2'] = nrm((DEPTH, SHARED_FF, D_MODEL), SHARED_FF ** -0.5 * DN_BETA)
    inp['w_ple_gate'] = nrm((DEPTH, D_MODEL, D_MODEL), D_MODEL ** -0.5)
    inp['w_ple'] = nrm((DEPTH, PLE_DIM, D_MODEL), PLE_DIM ** -0.5)
    inp['ln2_g'] = 1.0 + nrm((DEPTH, D_MODEL), 0.01)
    inp['ln2_b'] = nrm((DEPTH, D_MODEL), 0.01)
    return inp


def reference(x_prompt, x_sample, cache_kv_w128, cache_kv_w512, cache_kv_w2048, state_ssm, p_prompt, p_sample,
              w_in, a_re, a_im, log_dt, b_re, b_im, c_re, c_im, d_skip, w_glu, b_glu, w_attn_br, w_ssm_br,
              w_gate, b_gate, w_out, ln1_g, ln1_b, w_router, router_bias, w1, w3, w2, ws1, ws3, ws2,
              w_ple_gate, w_ple, ln2_g, ln2_b):
    caches = (cache_kv_w128, cache_kv_w512, cache_kv_w2048)
    pos_prompt = jnp.arange(x_prompt.shape[1], dtype=jnp.int32)
    pos_sample = PAST_LEN + jnp.arange(x_sample.shape[1], dtype=jnp.int32)
    yp, ys = x_prompt, x_sample
    kv_p = [[] for _ in range(N_GROUPS)]
    kv_s = [[] for _ in range(N_GROUPS)]
    h_p, h_s = [], []
    for l in range(DEPTH):
        w = {'w_in': w_in[l], 'a_re': a_re[l], 'a_im': a_im[l], 'log_dt': log_dt[l], 'b_re': b_re[l],
             'b_im': b_im[l], 'c_re': c_re[l], 'c_im': c_im[l], 'd_skip': d_skip[l], 'w_glu': w_glu[l],
             'b_glu': b_glu[l], 'w_attn_br': w_attn_br[l], 'w_ssm_br': w_ssm_br[l], 'w_gate': w_gate[l],
             'b_gate': b_gate[l], 'w_out': w_out[l], 'ln1_g': ln1_g[l], 'ln1_b': ln1_b[l],
             'w_router': w_router[l], 'router_bias': router_bias[l], 'w1': w1[l], 'w3': w3[l], 'w2': w2[l],
             'ws1': ws1[l], 'ws3': ws3[l], 'ws2': ws2[l], 'w_ple_gate': w_ple_gate[l], 'w_ple': w_ple[l],
             'ln2_g': ln2_g[l], 'ln2_b': ln2_b[l]}
        h0_p = jnp.zeros((yp.shape[0], SSM_GROUPS, SSM_STATE, 2), jnp.float32)
        yp, rows_p, hp = trunk_layer(yp, p_prompt[l], pos_prompt, attend_prompt, h0_p, w)
        attend_s = make_attend_sample([c[l] for c in caches])
        ys, rows_s, hs = trunk_layer(ys, p_sample[l], pos_sample, attend_s, state_ssm[l], w)
        for g in range(N_GROUPS):
            kv_p[g].append(rows_p[g])
            kv_s[g].append(rows_s[g])
        h_p.append(hp)
        h_s.append(hs)
    return (yp, ys, jnp.stack(kv_p[0]), jnp.stack(kv_s[0]), jnp.stack(kv_p[1]), jnp.stack(kv_s[1]),
            jnp.stack(kv_p[2]), jnp.stack(kv_s[2]), jnp.stack(h_p), jnp.stack(h_s))
```

```python
import math
from contextlib import ExitStack
import numpy as np
import concourse.bass as bass
import concourse.mybir as mybir
from concourse.bass_utils import run_bass_kernel_spmd

F32 = mybir.dt.float32
BF16 = mybir.dt.bfloat16
I32 = mybir.dt.int32
U32 = mybir.dt.uint32
AF = mybir.ActivationFunctionType
ALU = mybir.AluOpType
AX = mybir.AxisListType

ENGS = ("pe", "act", "dve", "pool", "sp")

S = 4096
NT = S // 128
NS = 16
D = 1024
CAP = 1024
NEXP = 64
ALPHA = 2.0 ** 0.25
LN_EPS = 1e-5
DILS = (1, 4, 16)
KEEP = (128, 512, 2048)
TWO_PI = 2.0 * math.pi


class Prog:
    def __init__(self, nc, stack, n_dma_sems=40):
        self.nc = nc
        self.ops = {e: [] for e in ENGS}
        self.cnt = {e: 0 for e in ENGS}
        self.sem = {e: stack.enter_context(nc.semaphore("c_" + e)) for e in ENGS}
        self.dsem = [stack.enter_context(nc.semaphore("d%d" % i)) for i in range(n_dma_sems)]
        self.dval = [0] * n_dma_sems
        self.dnext = 0
        self.seen = {e: {} for e in ENGS}
        self.res = {}
        self.ninst = 0
        self.swsem = [stack.enter_context(nc.semaphore("w%d" % i)) for i in range(8)]
        self.sw_pending = [None] * 8
        self.swval = [0] * 8
        self.sw_token = {}
        self.sw_next = 0
        self.sw_serial = 0
        self.sw_mark = stack.enter_context(nc.sbuf_tensor("sw_mark", [1, 8], F32))

    def _retire(self, j):
        return

    def _deps(self, reads, writes):
        deps = []
        for r in reads:
            st = self.res.get(r)
            if st and st["w"] is not None:
                deps.append(st["w"])
        for w in writes:
            st = self.res.get(w)
            if st:
                if st["w"] is not None:
                    deps.append(st["w"])
                deps.extend(st["r"])
        return deps

    def _emit_waits(self, eng, deps):
        need = {}
        for d in deps:
            if d[0] == "e":
                _, g, idx = d
                if g == eng and eng in ("pe", "sp"):
                    continue
                key = ("e", g)
                semh = self.sem[g]
            elif d[0] == "w":
                _, j, idx = d
                key = ("w", j)
                semh = self.swsem[j]
            else:
                _, j, idx = d
                key = ("d", j)
                semh = self.dsem[j]
            if self.seen[eng].get(key, 0) >= idx:
                continue
            if key not in need or need[key][1] < idx:
                need[key] = (semh, idx)
        for key, (semh, idx) in need.items():
            self.seen[eng][key] = idx
            self.ops[eng].append(lambda e, s=semh, v=idx: e.wait_ge(s, v))
            self.ninst += 1

    def _update(self, token, reads, writes):
        for r in reads:
            st = self.res.setdefault(r, {"w": None, "r": []})
            st["r"].append(token)
            if len(st["r"]) > 64:
                st["r"] = st["r"][-48:]
        for w in writes:
            self.res[w] = {"w": token, "r": []}

    def op(self, eng, fn, reads=(), writes=()):
        deps = self._deps(reads, writes)
        self._emit_waits(eng, deps)
        self.cnt[eng] += 1
        idx = self.cnt[eng]
        semh = self.sem[eng]
        self.ops[eng].append(lambda e, f=fn, s=semh: f(e).then_inc(s, 1))
        self.ninst += 1
        self._update(("e", eng, idx), reads, writes)
        return idx

    def dma(self, q, fn, reads=(), writes=()):
        if q == "pool":
            return self._dma_sw(fn, reads, writes)
        deps = self._deps(reads, writes)
        j = self.dnext
        self.dnext = (self.dnext + 1) % len(self.dsem)
        if self.dval[j] > 0:
            deps.append(("d", j, self.dval[j]))
        self._emit_waits(q, deps)
        self.dval[j] += 16
        val = self.dval[j]
        semh = self.dsem[j]
        self.ops[q].append(lambda e, f=fn, s=semh: f(e).then_inc(s, 16))
        self.ninst += 1
        tok = ("d", j, val)
        self._update(tok, reads, writes)
        return tok

    def _dma_sw(self, fn, reads, writes):
        j = self.sw_next
        self.sw_next = (self.sw_next + 1) % len(self.swsem)
        deps = self._deps(reads, writes)
        if self.swval[j] > 0:
            deps.append(("w", j, self.swval[j]))
        self._emit_waits("pool", deps)
        self.swval[j] += 16
        val = self.swval[j]
        semh = self.swsem[j]
        self.ops["pool"].append(lambda e, f=fn, s=semh: f(e).then_inc(s, 16))
        self.ninst += 1
        tok = ("w", j, val)
        self._update(tok, reads, writes)
        return tok

    def barrier(self):
        for j in range(len(self.swsem)):
            self._retire(j)
        deps = [("d", j, v) for j, v in enumerate(self.dval) if v > 0]
        deps += [("w", j, v) for j, v in enumerate(self.swval) if v > 0]
        deps += [("e", g, self.cnt[g]) for g in ENGS if self.cnt[g] > 0]
        for e in ENGS:
            self._emit_waits(e, deps)
        self.res = {}

    def flush(self):
        nc = self.nc
        ops = self.ops
        with nc.Block() as block:
            @block.tensor
            def _(e):
                for f in ops["pe"]:
                    f(e)

            @block.scalar
            def _(e):
                for f in ops["act"]:
                    f(e)

            @block.vector
            def _(e):
                for f in ops["dve"]:
                    f(e)

            @block.gpsimd
            def _(e):
                for f in ops["pool"]:
                    f(e)

            @block.sync
            def _(e):
                for f in ops["sp"]:
                    f(e)
        self.ops = {e: [] for e in ENGS}


class Rot:
    def __init__(self, name, bufs):
        self.name = name
        self.bufs = bufs
        self.i = 0

    def next(self):
        b = self.bufs[self.i % len(self.bufs)]
        r = "%s#%d" % (self.name, self.i % len(self.bufs))
        self.i += 1
        return b, r


def build(debug_stop=None):
    nc = bass.Bass("TRN2", target_bir_lowering=False)

    SLIM = ("x_p", "p_p", "p_s", "w_gate", "w_out", "w_attn_br", "w_ssm_br", "ws1", "ws2", "ws3", "w_ple_gate", "w_ple",
            "w_router", "c_rope", "st_ssm")

    SLIM2 = ("p_p", "p_s", "w_gate", "w_out", "w_attn_br", "w_ssm_br", "ws1", "ws2", "ws3", "w_ple_gate", "w_ple",
             "w_router", "st_ssm", "cache0", "cache1", "cache2", "x_s")
    bis = debug_stop if (debug_stop is not None and 1 < debug_stop < 2) else None

    def din(name, shape, dt=F32):
        if debug_stop is not None and debug_stop <= 0.5 and name in SLIM:
            shape = [1] * len(shape)
        if debug_stop is not None and 1 < debug_stop < 2 and name in SLIM2:
            shape = [1] * len(shape)
        return nc.dram_tensor(name, list(shape), dt, kind="ExternalInput").ap()

    def dout(name, shape, dt=F32):
        return nc.dram_tensor(name, list(shape), dt, kind="ExternalOutput").ap()

    def dscr(name, shape, dt=F32, dbg=False):
        if debug_stop is not None and dbg:
            return nc.dram_tensor(name, list(shape), dt, kind="ExternalOutput").ap()
        return nc.dram_tensor(name, list(shape), dt).ap()

    x_p = din("x_p", [S, D])
    x_s = din("x_s", [NS, D])
    cache = [din("cache%d" % g, [NS, KEEP[g], 512]) for g in range(3)]
    st_ssm = din("st_ssm", [NS, 2048])
    p_p = din("p_p", [S, 256])
    p_s = din("p_s", [NS, 256])
    w_in = din("w_in", [D, 2560])
    a_re = din("a_re", [16, 64]); a_im = din("a_im", [16, 64]); log_dt = din("log_dt", [16])
    b_re = din("b_re", [16, 64, 16]); b_im = din("b_im", [16, 64, 16])
    c_re = din("c_re", [16, 16, 64]); c_im = din("c_im", [16, 16, 64])
    d_skip = din("d_skip", [256]); w_glu = din("w_glu", [256, 256]); b_glu = din("b_glu", [256])
    w_attn_br = din("w_attn_br", [256, D]); w_ssm_br = din("w_ssm_br", [256, D])
    w_gate = din("w_gate", [D, 2 * D]); b_gate = din("b_gate", [2 * D])
    w_out = din("w_out", [D, D])
    ln1_g = din("ln1_g", [D]); ln1_b = din("ln1_b", [D])
    w_router = din("w_router", [D, NEXP]); router_bias = din("router_bias", [NEXP])
    NE_IN = NEXP if debug_stop is None or debug_stop >= 4 else 1
    w1 = din("w1", [NE_IN, D, 256]); w3 = din("w3", [NE_IN, D, 256]); w2 = din("w2", [NE_IN, 256, D])
    ws1 = din("ws1", [D, 256]); ws3 = din("ws3", [D, 256]); ws2 = din("ws2", [256, D])
    w_ple_gate = din("w_ple_gate", [D, D]); w_ple = din("w_ple", [256, D])
    ln2_g = din("ln2_g", [D]); ln2_b = din("ln2_b", [D])
    c_ident = din("c_ident", [128, 128])
    c_rope = din("c_rope", [3, S, 64])
    c_rope_s = din("c_rope_s", [1, 64])
    c_amask = din("c_amask", [128, 1024])
    c_utri = din("c_utri", [128, 128])
    c_iota = din("c_iota", [128, 64])
    c_sel = din("c_sel", [16, 128])
    c_selT = din("c_selT", [128, 16])

    y_p = dout("y_p", [S, D]); y_s = dout("y_s", [NS, D])
    kv_p = [dout("kv_p%d" % g, [KEEP[g], 512]) for g in range(3)]
    kv_s = [dout("kv_s%d" % g, [NS, 512]) for g in range(3)]
    ssm_p = dout("ssm_p", [1024, 2]); ssm_s = dout("ssm_s", [NS, 2048])

    nd_scr = [dscr("nd_scr%d" % g, [S, 260], dbg=True) for g in range(3)]
    x1_scr = dscr("x1_scr", [S + NS, D])
    r_scr = dscr("r_scr", [S + NS, D])
    xg_scr = dscr("xg_scr", [NEXP * CAP, D], BF16)
    yg_scr = dscr("yg_scr", [NEXP * CAP, D])

    top = ExitStack()
    with top:
        P = Prog(nc, top)

        def sb(stack, name, shape, dt):
            return stack.enter_context(nc.sbuf_tensor(name, list(shape), dt))

        def psum(stack, name, shape, dt):
            return stack.enter_context(nc.psum_tensor(name, list(shape), dt))

        def MM(out, lhsT, rhs, start, stop, reads, writes):
            P.op("pe", lambda e: e.matmul(out, lhsT=lhsT, rhs=rhs, start=start, stop=stop), reads, writes)

        def TR(out, in_, ident, reads, writes):
            P.op("pe", lambda e: e.transpose(out=out, in_=in_, identity=ident), reads, writes)

        def ACT(out, in_, func, reads, writes, bias=None, scale=None, eng="act"):
            kw = {}
            if bias is not None:
                kw["bias"] = bias
            if scale is not None:
                kw["scale"] = scale
            P.op("act", lambda e: e.activation(out=out, in_=in_, func=func, **kw), reads, writes)

        def TT(eng, out, in0, in1, op, reads, writes):
            P.op(eng, lambda e: e.tensor_tensor(out=out, in0=in0, in1=in1, op=op), reads, writes)

        def TS(eng, out, in0, s1, s2, op0, op1, reads, writes):
            if op1 is None:
                P.op(eng, lambda e: e.tensor_scalar(out=out, in0=in0, scalar1=s1, scalar2=None, op0=op0), reads, writes)
            else:
                P.op(eng, lambda e: e.tensor_scalar(out=out, in0=in0, scalar1=s1, scalar2=s2, op0=op0, op1=op1), reads, writes)

        def STT(eng, out, in0, scalar, in1, op0, op1, reads, writes):
            P.op(eng, lambda e: e.scalar_tensor_tensor(out=out, in0=in0, scalar=scalar, in1=in1, op0=op0, op1=op1), reads, writes)

        def CP(eng, out, in_, reads, writes):
            if eng == "act":
                P.op("act", lambda e: e.activation(out=out, in_=in_, func=AF.Copy), reads, writes)
            else:
                P.op(eng, lambda e: e.tensor_copy(out=out, in_=in_), reads, writes)

        def MSET(eng, ap, val, writes):
            P.op(eng, lambda e: e.memset(ap, val), (), writes)

        def DMA(q, out, in_, reads, writes, **kw):
            return P.dma(q, lambda e: e.dma_start(out=out, in_=in_, **kw), reads, writes)

        ident_f = sb(top, "ident_f", [128, 128], F32)
        ident_b = sb(top, "ident_b", [128, 128], BF16)
        vecs = sb(top, "vecs", [128, 20], F32)
        rbias_t = sb(top, "rbias_t", [128, NEXP], F32)
        dest_t = sb(top, "dest_t", [128, NT + 1, 8], I32)
        gate_t = sb(top, "gate_t", [128, NT + 1, 8], F32)
        eps_t = sb(top, "eps_t", [128, 1], F32)

        DMA("sp", ident_f[:], c_ident, (), ["ident_f"])
        CP("dve", ident_b[:], ident_f[:], ["ident_f"], ["ident_b"])
        DMA("sp", rbias_t[:], router_bias.partition_broadcast(128), (), ["rbias"])
        MSET("dve", eps_t[:], LN_EPS, ["eps"])
        MSET("dve", gate_t[:], 0.0, ["gate_t"])
        MSET("dve", dest_t[:], 2000000, ["dest_t"])

        stB = top.enter_context(ExitStack())
        uT = sb(stB, "uT", [128, 2, S], BF16)
        uT_s = sb(stB, "uT_s", [128, 2, NS], BF16)
        bbT = [sb(stB, "bbT%d" % i, [128, 8, 128], BF16) for i in range(2)]
        cT = [sb(stB, "cT%d" % i, [128, 8, 128], BF16) for i in range(2)]
        diagD = sb(stB, "diagD", [128, 2, 128], BF16)
        par = sb(stB, "par", [128, 16, 8], F32)
        carry = sb(stB, "carry", [128, 8, 2], F32)
        attn_oT_s = sb(stB, "attn_oT_s", [128, 2, NS], BF16)

        AR, AI, LDT, DT, ARDT, TH, RHO, COS, SIN, ABR, ABI, CRE, CIM, T0, T1, T2 = range(16)

        def pr(i):
            return par[:, i, :]

        with ExitStack() as st0:
            stage = sb(st0, "stage", [20, 128], F32)
            stage2 = sb(st0, "stage2", [8, 3, 128], F32)
            ldt8 = sb(st0, "ldt8", [8, 2], F32)
            pz = psum(st0, "pz", [128, 4, 128], F32)
            pz2 = psum(st0, "pz2", [128, 512], F32)
            bre = sb(st0, "bre", [128, 8, 16], F32); bim = sb(st0, "bim", [128, 8, 16], F32)
            bbr = sb(st0, "bbr", [128, 8, 16], F32); bbi = sb(st0, "bbi", [128, 8, 16], F32)
            tb1 = sb(st0, "tb1", [128, 8, 16], F32); tb2 = sb(st0, "tb2", [128, 8, 16], F32)
            Yst = [sb(st0, "Yst%d" % i, [128, 8, 128], F32) for i in range(2)]
            Xc = [sb(st0, "Xc%d" % i, [128, 8, 128], F32) for i in range(2)]
            qi_t = sb(st0, "qi_t", [128, 8], I32)

            DMA("sp", stage[0:2, :], d_skip.rearrange("(n p) -> n p", p=128), (), ["stage"])
            DMA("sp", stage[2:4, :], b_glu.rearrange("(n p) -> n p", p=128), (), ["stage"])
            DMA("sp", stage[4:20, :], b_gate.rearrange("(n p) -> n p", p=128), (), ["stage"])
            TR(pz2[:, 0:20], stage[:, :], ident_f[0:20, 0:20], ["stage", "ident_f"], ["pz2"])
            CP("dve", vecs[:], pz2[:, 0:20], ["pz2"], ["vecs"])

            DMA("sp", stage2[:, 0, :], a_re.rearrange("(st gl) p -> st (gl p)", gl=2), (), ["stage2a"])
            DMA("sp", stage2[:, 1, :], a_im.rearrange("(st gl) p -> st (gl p)", gl=2), (), ["stage2b"])
            DMA("sp", ldt8[:], log_dt.rearrange("(st gl) -> st gl", gl=2), (), ["ldt8"])
            CP("dve", stage2[:, 2, :].rearrange("s (gl p) -> s gl p", gl=2), ldt8[:].unsqueeze(2).to_broadcast([8, 2, 64]),
               ["ldt8"], ["stage2c"])
            for i in range(3):
                TR(pz2[:, 32 + 8 * i:40 + 8 * i], stage2[:, i, :], ident_f[0:8, 0:8],
                   ["stage2a", "stage2b", "stage2c", "ident_f"], ["pz2"])
            CP("dve", par[:, 0:3, :], pz2[:, 32:56].rearrange("p (a b) -> p a b", a=3), ["pz2"], ["par"])
            R = ["par"]
            ACT(pr(DT), pr(LDT), AF.Exp, R, R)
            TT("dve", pr(ARDT), pr(AR), pr(DT), ALU.mult, R, R)
            TT("dve", pr(TH), pr(AI), pr(DT), ALU.mult, R, R)
            ACT(pr(RHO), pr(ARDT), AF.Exp, R, R)

            def sin_of(dst, src_row, shift):
                TS("dve", pr(T0), pr(src_row), shift, 1.0 / TWO_PI, ALU.add, ALU.mult, R, R)
                CP("dve", qi_t[:], pr(T0), R, ["qi_t"])
                CP("dve", pr(T1), qi_t[:], ["qi_t"], R)
                TS("dve", pr(T0), pr(src_row), shift, None, ALU.add, None, R, R)
                STT("dve", pr(T0), pr(T1), -TWO_PI, pr(T0), ALU.mult, ALU.add, R, R)
                P.op("dve", lambda e: e.tensor_single_scalar(out=pr(T1), in_=pr(T0), scalar=math.pi, op=ALU.is_gt), R, R)
                STT("dve", pr(T0), pr(T1), -TWO_PI, pr(T0), ALU.mult, ALU.add, R, R)
                P.op("dve", lambda e: e.tensor_single_scalar(out=pr(T1), in_=pr(T0), scalar=-math.pi, op=ALU.is_lt), R, R)
                STT("dve", pr(T0), pr(T1), TWO_PI, pr(T0), ALU.mult, ALU.add, R, R)
                ACT(dst, pr(T0), AF.Sin, R, R)

            sin_of(pr(SIN), TH, 0.0)
            sin_of(pr(COS), TH, math.pi / 2.0)
            TT("dve", pr(ABR), pr(RHO), pr(COS), ALU.mult, R, R)
            TT("dve", pr(ABI), pr(RHO), pr(SIN), ALU.mult, R, R)
            TT("dve", pr(T0), pr(AR), pr(AR), ALU.mult, R, R)
            TT("dve", pr(T1), pr(AI), pr(AI), ALU.mult, R, R)
            TT("dve", pr(T0), pr(T0), pr(T1), ALU.add, R, R)
            P.op("dve", lambda e: e.reciprocal(out=pr(T2), in_=pr(T0)), R, R)
            TS("dve", pr(T0), pr(ABR), -1.0, None, ALU.add, None, R, R)
            TT("dve", pr(CRE), pr(T0), pr(AR), ALU.mult, R, R)
            TT("dve", pr(T1), pr(ABI), pr(AI), ALU.mult, R, R)
            TT("dve", pr(CRE), pr(CRE), pr(T1), ALU.add, R, R)
            TT("dve", pr(CRE), pr(CRE), pr(T2), ALU.mult, R, R)
            TT("dve", pr(CIM), pr(ABI), pr(AR), ALU.mult, R, R)
            TT("dve", pr(T1), pr(T0), pr(AI), ALU.mult, R, R)
            TT("dve", pr(CIM), pr(CIM), pr(T1), ALU.subtract, R, R)
            TT("dve", pr(CIM), pr(CIM), pr(T2), ALU.mult, R, R)

            DMA("sp", bre[:], b_re.rearrange("(st gl) p c -> (gl p) st c", gl=2), (), ["bre"])
            DMA("sp", bim[:], b_im.rearrange("(st gl) p c -> (gl p) st c", gl=2), (), ["bim"])
            cre_b = pr(CRE).unsqueeze(2).to_broadcast([128, 8, 16])
            cim_b = pr(CIM).unsqueeze(2).to_broadcast([128, 8, 16])
            TT("dve", tb1[:], bre[:], cre_b, ALU.mult, ["bre"] + R, ["tb1"])
            TT("dve", tb2[:], bim[:], cim_b, ALU.mult, ["bim"] + R, ["tb2"])
            TT("dve", bbr[:], tb1[:], tb2[:], ALU.subtract, ["tb1", "tb2"], ["bbr"])
            TT("dve", tb1[:], bim[:], cre_b, ALU.mult, ["bim"] + R, ["tb1"])
            TT("dve", tb2[:], bre[:], cim_b, ALU.mult, ["bre"] + R, ["tb2"])
            TT("dve", bbi[:], tb1[:], tb2[:], ALU.add, ["tb1", "tb2"], ["bbi"])
            for i, src in enumerate((bbr, bbi)):
                MSET("pool", Yst[i][:], 0.0, ["Yst%d" % i])
                for st_ in range(8):
                    for gl in range(2):
                        off = 32 * (st_ % 4) + 16 * gl
                        CP("dve", Yst[i][64 * gl:64 * gl + 64, st_, off:off + 16], src[64 * gl:64 * gl + 64, st_, :],
                           ["bbr", "bbi", "Yst%d" % i], ["Yst%d" % i])
                for half in range(2):
                    for j in range(4):
                        st_ = 4 * half + j
                        TR(pz[:, j, :], Yst[i][:, st_, :], ident_f[:], ["Yst%d" % i, "ident_f"], ["pz"])
                    CP("act", bbT[i][:, 4 * half:4 * half + 4, :], pz[:], ["pz"], ["bbT%d" % i])
            for i, src in enumerate((c_re, c_im)):
                MSET("pool", Xc[i][:], 0.0, ["Xc%d" % i])
                for st_ in range(8):
                    for gl in range(2):
                        off = 32 * (st_ % 4) + 16 * gl
                        DMA("sp", Xc[i][off:off + 16, st_, 64 * gl:64 * gl + 64], src[2 * st_ + gl], ["Xc%d" % i], ["Xc%d" % i])
                for half in range(2):
                    for j in range(4):
                        st_ = 4 * half + j
                        TR(pz[:, j, :], Xc[i][:, st_, :], ident_f[:], ["Xc%d" % i, "ident_f"], ["pz"])
                    ACT(cT[i][:, 4 * half:4 * half + 4, :], pz[:], AF.Copy, ["pz"], ["cT%d" % i], scale=(1.0 if i == 0 else -1.0))
            for ck in range(2):
                TS("dve", diagD[:, ck, :], ident_f[:], vecs[:, ck:ck + 1], None, ALU.mult, None, ["ident_f", "vecs"], ["diagD"])
            MSET("dve", carry[:], 0.0, ["carry"])
            if debug_stop == 0:
                dbg_par = dout("dbg_par", [128, 16 * 8])
                dbg_bbT = dout("dbg_bbT", [128, 4, 8 * 128], BF16)
                DMA("sp", dbg_par, par[:].rearrange("p a b -> p (a b)"), ["par"], ["dbg_par"])
                for i_, t_ in enumerate((bbT[0], bbT[1], cT[0], cT[1])):
                    DMA("sp", dbg_bbT[:, i_, :], t_[:].rearrange("p a b -> p (a b)"), ["bbT0", "bbT1", "cT0", "cT1"], ["dbg_bbT"])
            P.barrier()
            P.flush()
            if debug_stop == 0:
                return nc, {}

        with ExitStack() as st1:
            w_in_b = sb(st1, "w_in_b", [128, 8, 2560], BF16)
            w_in_v = w_in.rearrange("(kc p) n -> p kc n", p=128)
            for g in range(3):
                for part in range(3):
                    DMA("pool", w_in_b[:, :, 768 * g + 256 * part:768 * g + 256 * part + 256],
                        w_in_v[:, :, 768 * part + 256 * g:768 * part + 256 * g + 256], (), ["w_in_b"])
            DMA("pool", w_in_b[:, :, 2304:2560], w_in_v[:, :, 2304:2560], (), ["w_in_b"])
            amask = sb(st1, "amask", [128, 1024], BF16)
            nd_s = sb(st1, "nd_s", [NS, 3, 260], F32)

            with ExitStack() as sts:
              if not bis:
                  xs_f = sb(sts, "xs_f", [NS, D], F32)
                  xs_b = sb(sts, "xs_b", [NS, D], BF16)
                  xsT = sb(sts, "xsT", [128, 8, NS], BF16)
                  pj = psum(sts, "pj_s", [NS, 5, 512], F32)
                  pt_full = psum(sts, "pt_s", [128, 1024], BF16)
                  pt = pt_full[:, 0:8 * NS].rearrange("p (a b) -> p a b", a=8)
                  pq_full = psum(sts, "pq_s", [128, 512], F32)
                  pq = pq_full[:, 0:256]
                  pn_full = psum(sts, "pn_s", [128, 512], F32)
                  pn = pn_full[0:NS, 0:260]
                  rope_s = sb(sts, "rope_s", [NS, 64], F32)
                  qk_s = sb(sts, "qk_s", [NS, 3, 8, 64], F32)
                  v_s = sb(sts, "v_s", [NS, 3, 256], F32)
                  u_sb = sb(sts, "u_sb", [NS, 256], BF16)
                  kvo = sb(sts, "kvo", [NS, 3, 512], F32)
                  tq = [sb(sts, "tq%d" % i, [NS, 1, 8, 32], F32) for i in range(4)]
                  sel = sb(sts, "sel", [16, 128], F32)
                  selT = sb(sts, "selT", [128, 16], F32)
                  ctile = sb(sts, "ctile", [128, 16, 512], F32)
                  prod = sb(sts, "prod", [128, 16, 256], F32)
                  qb = sb(sts, "qb", [128, 256], F32)
                  lg = sb(sts, "lg", [128, 16, 4], F32)
                  pex = sb(sts, "pex", [128, 16, 4], F32)
                  part = sb(sts, "part", [128, 4, 65], F32)
                  lgn = sb(sts, "lgn", [NS, 4], F32)
                  pnew = sb(sts, "pnew", [NS, 4], F32)
                  tn = sb(sts, "tn", [NS, 4, 64], F32)
                  rden = sb(sts, "rden", [NS, 4], F32)
                  o_s = sb(sts, "o_s", [NS, 4, 64], F32)
                  o_sb = sb(sts, "o_sb", [NS, 256], BF16)

                  DMA("sp", xs_f[:], x_s, (), ["xs_f"])
                  DMA("sp", rope_s[:], c_rope_s[0].partition_broadcast(NS), (), ["rope_s"])
                  DMA("sp", sel[:], c_sel, (), ["sel"])
                  DMA("sp", selT[:], c_selT, (), ["selT"])
                  selb = sb(sts, "selb", [16, 128], BF16)
                  selTb = sb(sts, "selTb", [128, 16], BF16)
                  q_hi = sb(sts, "q_hi", [NS, 256], BF16); q_lo = sb(sts, "q_lo", [NS, 256], BF16)
                  p_hi = sb(sts, "p_hi", [128, 260], BF16); p_lo = sb(sts, "p_lo", [128, 260], BF16)
                  CP("dve", selb[:], sel[:], ["sel"], ["selb"])
                  CP("dve", selTb[:], selT[:], ["selT"], ["selTb"])
                  if debug_stop == 0.21:
                      P.barrier()
                      P.flush()
                      return nc, {}
                  CP("act", xs_b[:], xs_f[:], ["xs_f"], ["xs_b"])
                  for kc in range(8):
                      TR(pt[:, kc, :], xs_b[:, kc * 128:(kc + 1) * 128], ident_b[0:NS, 0:NS], ["xs_b", "ident_b"], ["pt_s"])
                  CP("dve", xsT[:], pt, ["pt_s"], ["xsT"])
                  if debug_stop == 0.22:
                      P.barrier()
                      P.flush()
                      return nc, {}
                  for ch in range(5):
                      for kc in range(8):
                          MM(pj[:, ch, :], xsT[:, kc, :], w_in_b[:, kc, 512 * ch:512 * ch + 512], kc == 0, kc == 7,
                             ["xsT", "w_in_b"], ["pj_s"])
                  pj_sb = sb(sts, "pj_sb", [NS, 2560], F32)
                  for ch in range(5):
                      CP("act" if ch % 2 else "dve", pj_sb[:, 512 * ch:512 * ch + 512], pj[:, ch, :], ["pj_s"], ["pj_s"])
                  pjf = pj_sb[:]
                  if debug_stop == 0.23:
                      P.barrier()
                      P.flush()
                      return nc, {}
                  for g in range(3):
                      qk = pjf[:, 768 * g:768 * g + 512].rearrange("p (h d) -> p h d", h=8)
                      x1 = qk[:, :, 0:32]; x2 = qk[:, :, 32:64]
                      cs = rope_s[:, 0:32].unsqueeze(1).to_broadcast([NS, 8, 32])
                      sn = rope_s[:, 32:64].unsqueeze(1).to_broadcast([NS, 8, 32])
                      TT("dve", tq[0][:, 0], x1, cs, ALU.mult, ["pj_s", "rope_s"], ["tq0"])
                      TT("dve", tq[1][:, 0], x2, sn, ALU.mult, ["pj_s", "rope_s"], ["tq1"])
                      TT("dve", tq[2][:, 0], x2, cs, ALU.mult, ["pj_s", "rope_s"], ["tq2"])
                      TT("dve", tq[3][:, 0], x1, sn, ALU.mult, ["pj_s", "rope_s"], ["tq3"])
                      TT("dve", qk_s[:, g, :, 0:32], tq[0][:, 0], tq[1][:, 0], ALU.subtract, ["tq0", "tq1"], ["qk_s"])
                      TT("dve", qk_s[:, g, :, 32:64], tq[2][:, 0], tq[3][:, 0], ALU.add, ["tq2", "tq3"], ["qk_s"])
                      CP("act", v_s[:, g, :], pjf[:, 768 * g + 512:768 * g + 768], ["pj_s"], ["v_s"])
                      CP("dve", kvo[:, g, 0:256], qk_s[:, g, 4:8, :].rearrange("p h d -> p (h d)"), ["qk_s"], ["kvo"])
                      CP("dve", kvo[:, g, 256:512], v_s[:, g, :], ["v_s"], ["kvo"])
                      DMA("sp", kv_s[g], kvo[:, g, :], ["kvo"], ["kv_s%d" % g])
                  if debug_stop == 0.24:
                      P.barrier()
                      P.flush()
                      return nc, {}
                  CP("act", u_sb[:], pjf[:, 2304:2560], ["pj_s"], ["u_sb"])
                  for ck in range(2):
                      TR(pt[:, ck, :], u_sb[:, ck * 128:(ck + 1) * 128], ident_b[0:NS, 0:NS], ["u_sb", "ident_b"], ["pt_s"])
                  CP("dve", uT_s[:], pt[:, 0:2, :], ["pt_s"], ["uT_s"])
                  if debug_stop == 0.25:
                      P.barrier()
                      P.flush()
                      return nc, {}

                  for g in range(3):
                      dil = DILS[g]
                      src = cache[g].rearrange("s (c kk dd) f -> (s c) kk dd f", c=8, kk=16, dd=dil)[:, :, 0, :]
                      DMA("sp", ctile[:], src, ["ctile"], ["ctile"])
                      qg = qk_s[:, g, 0:4, :].rearrange("p h d -> p (h d)")
                      CP("dve", q_hi[:], qg, ["qk_s"], ["q_hi"])
                      TT("dve", q_lo[:], qg, q_hi[:], ALU.subtract, ["qk_s", "q_hi"], ["q_lo"])
                      MM(pq, selb[:], q_hi[:], True, False, ["selb", "q_hi"], ["pq_s"])
                      MM(pq, selb[:], q_lo[:], False, True, ["selb", "q_lo"], ["pq_s"])
                      CP("act", qb[:], pq, ["pq_s"], ["qb"])
                      TT("dve", prod[:], ctile[:, :, 0:256], qb[:].unsqueeze(1).to_broadcast([128, 16, 256]), ALU.mult,
                         ["ctile", "qb"], ["prod"])
                      P.op("dve", lambda e: e.tensor_reduce(out=lg[:], in_=prod[:].rearrange("p k (h d) -> p k h d", h=4),
                                                            axis=AX.X, op=ALU.add), ["prod"], ["lg"])
                      ACT(pex[:], lg[:], AF.Exp, ["lg"], ["pex"], scale=0.125)
                      TT("dve", prod[:].rearrange("p k (h d) -> p k h d", h=4),
                         ctile[:, :, 256:512].rearrange("p k (h d) -> p k h d", h=4),
                         pex[:].unsqueeze(3).to_broadcast([128, 16, 4, 64]), ALU.mult, ["ctile", "pex", "prod"], ["prod"])
                      P.op("dve", lambda e: e.tensor_reduce(out=part[:, :, 0:64], in_=prod[:].rearrange("p k (h d) -> p h d k", h=4),
                                                            axis=AX.X, op=ALU.add), ["prod"], ["part"])
                      P.op("dve", lambda e: e.tensor_reduce(out=part[:, :, 64], in_=pex[:].rearrange("p k h -> p h k"),
                                                            axis=AX.X, op=ALU.add), ["pex", "part"], ["part"])
                      partf = part[:].rearrange("p h d -> p (h d)")
                      CP("dve", p_hi[:], partf, ["part"], ["p_hi"])
                      TT("dve", p_lo[:], partf, p_hi[:], ALU.subtract, ["part", "p_hi"], ["p_lo"])
                      MM(pn, selTb[:], p_hi[:], True, False, ["selTb", "p_hi"], ["pn_s"])
                      MM(pn, selTb[:], p_lo[:], False, True, ["selTb", "p_lo"], ["pn_s"])
                      TT("dve", tn[:], qk_s[:, g, 0:4, :], qk_s[:, g, 4:8, :], ALU.mult, ["qk_s"], ["tn"])
                      P.op("dve", lambda e: e.tensor_reduce(out=lgn[:], in_=tn[:], axis=AX.X, op=ALU.add), ["tn"], ["lgn"])
                      ACT(pnew[:], lgn[:], AF.Exp, ["lgn"], ["pnew"], scale=0.125)
                      TT("dve", tn[:], v_s[:, g, :].rearrange("p (h d) -> p h d", h=4),
                         pnew[:].unsqueeze(2).to_broadcast([NS, 4, 64]), ALU.mult, ["v_s", "pnew", "tn"], ["tn"])
                      pnv = pn.rearrange("p (h d) -> p h d", h=4)
                      ndv = nd_s[:, g, :].rearrange("p (h d) -> p h d", h=4)
                      TT("dve", ndv[:, :, 0:64], pnv[:, :, 0:64], tn[:], ALU.add, ["pn_s", "tn"], ["nd_s"])
                      TT("dve", ndv[:, :, 64:65], pnv[:, :, 64:65], pnew[:].unsqueeze(2), ALU.add, ["pn_s", "pnew", "nd_s"], ["nd_s"])
                  nds = nd_s[:, 0, :]
                  TT("dve", nds, nd_s[:, 0, :], nd_s[:, 1, :], ALU.add, ["nd_s"], ["nd_s"])
                  TT("dve", nds, nd_s[:, 0, :], nd_s[:, 2, :], ALU.add, ["nd_s"], ["nd_s"])
                  ndv = nd_s[:, 0, :].rearrange("p (h d) -> p h d", h=4)
                  P.op("dve", lambda e: e.reciprocal(out=rden[:], in_=ndv[:, :, 64:65].rearrange("p h o -> p (h o)")), ["nd_s"], ["rden"])
                  TT("dve", o_s[:], ndv[:, :, 0:64], rden[:].unsqueeze(2).to_broadcast([NS, 4, 64]), ALU.mult, ["nd_s", "rden"], ["o_s"])
                  CP("act", o_sb[:], o_s[:].rearrange("p h d -> p (h d)"), ["o_s"], ["o_sb"])
                  for pr_ in range(2):
                      TR(pt[:, pr_, :], o_sb[:, pr_ * 128:(pr_ + 1) * 128], ident_b[0:NS, 0:NS], ["o_sb", "ident_b"], ["pt_s"])
                  CP("dve", attn_oT_s[:], pt[:, 0:2, :], ["pt_s"], ["attn_oT_s"])
                  if debug_stop == 0.5:
                      dbg_nds = dout("dbg_nds", [NS, 3 * 260])
                      DMA("sp", dbg_nds, nd_s[:].rearrange("p a b -> p (a b)"), ["nd_s"], ["dbg_nds"])
                  P.barrier()
                  P.flush()
                  if debug_stop == 0.5:
                      return nc, {}

            with ExitStack() as stp:
                kT_g = sb(stp, "kT_g", [128, 2, S], BF16)
                V_g = sb(stp, "V_g", [128, NT, 4, 65], BF16)
                NB = 2
                x_f = Rot("x_f", [sb(stp, "x_f%d" % i, [128, D], F32) for i in range(NB)])
                x_b = Rot("x_b", [sb(stp, "x_b%d" % i, [128, D], BF16) for i in range(NB)])
                xTt = Rot("xTt", [sb(stp, "xTt%d" % i, [128, 8, 128], BF16) for i in range(NB)])
                ropet = Rot("ropet", [sb(stp, "ropet%d" % i, [128, 64], F32) for i in range(NB)])
                tr_ = [Rot("tr%d" % j, [sb(stp, "tr%d_%d" % (j, i), [128, 8, 32], F32) for i in range(NB)]) for j in range(4)]
                qkr = Rot("qkr", [sb(stp, "qkr%d" % i, [128, 8, 64], F32) for i in range(NB)])
                qkb = Rot("qkb", [sb(stp, "qkb%d" % i, [128, 512], BF16) for i in range(NB)])
                qTt = Rot("qTt", [sb(stp, "qTt%d" % i, [128, 2, 128], BF16) for i in range(NB)])
                vf = Rot("vf", [sb(stp, "vf%d" % i, [128, 256], F32) for i in range(NB)])
                kvt = Rot("kvt", [sb(stp, "kvt%d" % i, [128, 512], F32) for i in range(NB)])
                ub = Rot("ub", [sb(stp, "ub%d" % i, [128, 256], BF16) for i in range(NB)])
                pe_sb = Rot("pe_sb", [sb(stp, "pe_sb%d" % i, [128, 1024], BF16) for i in range(NB)])
                pm_sb = Rot("pm_sb", [sb(stp, "pm_sb%d" % i, [128, 1024], BF16) for i in range(NB)])
                ndt = Rot("ndt", [sb(stp, "ndt%d" % i, [128, 260], F32) for i in range(NB)])
                p_xT = psum(stp, "p_xT", [128, 8, 128], BF16)
                p_qk = psum(stp, "p_qk", [128, 512], F32)
                p_vu = psum(stp, "p_vu", [128, 512], F32)
                p_t = psum(stp, "p_t", [128, 8, 128], BF16)
                p_S = psum(stp, "p_S", [128, 1024], F32)
                p_O_full = psum(stp, "p_O", [128, 512], F32)
                p_O = p_O_full[:, 0:260].rearrange("p (h d) -> p h d", h=4)
                MSET("pool", V_g[:, :, :, 64:65], 1.0, ["V_ones"])
                amask_f = sb(stp, "amask_f", [128, 1024], F32)
                DMA("sp", amask_f[:], c_amask, (), ["amask_f"])
                CP("dve", amask[:], amask_f[:], ["amask_f"], ["amask"])

                for g in range(3):
                    dil = DILS[g]
                    tpc = NT // dil
                    for n in range(NT):
                        r, c = divmod(n, tpc)
                        if bis and (g > 0 or n not in (0, 1, 31)):
                            continue
                        rows = slice(r + 128 * c * dil, r + 128 * c * dil + 127 * dil + 1, dil)
                        xf, xf_r = x_f.next()
                        DMA("sp", xf[:], x_p[rows, :], [xf_r], [xf_r])
                        rt, rt_r = ropet.next()
                        DMA("sp", rt[:], c_rope[g, 128 * n:128 * n + 128, :], [rt_r], [rt_r])
                        xb, xb_r = x_b.next()
                        CP("act", xb[:], xf[:], [xf_r], [xb_r])
                        for kc in range(8):
                            TR(p_xT[:, kc, :], xb[:, kc * 128:(kc + 1) * 128], ident_b[:], [xb_r, "ident_b"], ["p_xT"])
                        xt, xt_r = xTt.next()
                        CP("dve", xt[:], p_xT[:], ["p_xT"], [xt_r])
                        for kc in range(8):
                            MM(p_qk[:], xt[:, kc, :], w_in_b[:, kc, 768 * g:768 * g + 512], kc == 0, kc == 7,
                               [xt_r, "w_in_b"], ["p_qk"])
                        nv = 512 if g == 0 else 256
                        for kc in range(8):
                            if g == 0:
                                MM(p_vu[:, 0:256], xt[:, kc, :], w_in_b[:, kc, 512:768], kc == 0, kc == 7,
                                   [xt_r, "w_in_b"], ["p_vu"])
                            else:
                                MM(p_vu[:, 0:256], xt[:, kc, :], w_in_b[:, kc, 768 * g + 512:768 * g + 768], kc == 0, kc == 7,
                                   [xt_r, "w_in_b"], ["p_vu"])
                        if g == 0:
                            for kc in range(8):
                                MM(p_vu[:, 256:512], xt[:, kc, :], w_in_b[:, kc, 2304:2560], kc == 0, kc == 7,
                                   [xt_r, "w_in_b"], ["p_vu"])
                        if bis and bis <= 1.1:
                            continue
                        qk = p_qk[:].rearrange("p (h d) -> p h d", h=8)
                        x1 = qk[:, :, 0:32]; x2 = qk[:, :, 32:64]
                        cs = rt[:, 0:32].unsqueeze(1).to_broadcast([128, 8, 32])
                        sn = rt[:, 32:64].unsqueeze(1).to_broadcast([128, 8, 32])
                        t = [tr_[j].next() for j in range(4)]
                        TT("dve", t[0][0][:], x1, cs, ALU.mult, ["p_qk", rt_r], [t[0][1]])
                        TT("dve", t[1][0][:], x2, sn, ALU.mult, ["p_qk", rt_r], [t[1][1]])
                        TT("dve", t[2][0][:], x2, cs, ALU.mult, ["p_qk", rt_r], [t[2][1]])
                        TT("dve", t[3][0][:], x1, sn, ALU.mult, ["p_qk", rt_r], [t[3][1]])
                        qr, qr_r = qkr.next()
                        TT("pool", qr[:, :, 0:32], t[0][0][:], t[1][0][:], ALU.subtract, [t[0][1], t[1][1]], [qr_r])
                        TT("pool", qr[:, :, 32:64], t[2][0][:], t[3][0][:], ALU.add, [t[2][1], t[3][1], qr_r], [qr_r])
                        qb_, qb_r = qkb.next()
                        CP("act", qb_[:], qr[:].rearrange("p h d -> p (h d)"), [qr_r], [qb_r])
                        CP("act", V_g[:, n, :, 0:64], p_vu[:, 0:256].rearrange("p (h d) -> p h d", h=4), ["p_vu"], ["V_g%d" % n])
                        if bis and bis <= 1.2:
                            continue
                        is_out = (c == tpc - 1)
                        if is_out:
                            kv, kv_r = kvt.next()
                            CP("act", kv[:, 256:512], p_vu[:, 0:256], ["p_vu"], [kv_r])
                            CP("pool", kv[:, 0:256], qr[:, 4:8, :].rearrange("p h d -> p (h d)"), [qr_r, kv_r], [kv_r])
                            orow = slice(r, r + 127 * dil + 1, dil)
                            DMA("sp", kv_p[g][orow, :], kv[:], [kv_r], ["kv_p%d" % g])
                        if g == 0:
                            u_, u_r = ub.next()
                            CP("act", u_[:], p_vu[:, 256:512], ["p_vu"], [u_r])
                        if bis and bis <= 1.3:
                            continue
                        for j in range(4):
                            TR(p_t[:, j, :], qb_[:, j * 128:(j + 1) * 128], ident_b[:], [qb_r, "ident_b"], ["p_t"])
                        if g == 0:
                            for j in range(2):
                                TR(p_t[:, 4 + j, :], u_[:, j * 128:(j + 1) * 128], ident_b[:], [u_r, "ident_b"], ["p_t"])
                        if bis and bis <= 1.32:
                            continue
                        qt, qt_r = qTt.next()
                        CP("dve", qt[:], p_t[:, 0:2, :], ["p_t"], [qt_r])
                        if bis and bis <= 1.34:
                            continue
                        CP("dve", kT_g[:, :, 128 * n:128 * n + 128], p_t[:, 2:4, :], ["p_t"], ["kT_g%d" % n])
                        if bis and bis <= 1.36:
                            continue
                        if g == 0:
                            CP("dve", uT[:, :, 128 * n:128 * n + 128], p_t[:, 4:6, :], ["p_t"], ["uT%d" % n])
                        if bis and bis <= 1.4:
                            continue
                        kts = ([n - 1] if c > 0 else []) + [n]
                        pSv = p_S[:].rearrange("p (h kt q) -> p h kt q", h=4, kt=2)
                        for h in range(4):
                            for kt_n in kts:
                                kti = 1 if kt_n == n else 0
                                po_ = 64 * (h % 2)
                                MM(pSv[:, 2 * (h % 2) + h // 2, kti, :], kT_g[po_:po_ + 64, h // 2, 128 * kt_n:128 * kt_n + 128],
                                   qt[po_:po_ + 64, h // 2, :], True, True, ["kT_g%d" % kt_n, qt_r], ["p_S"])
                        pe_, pe_r = pe_sb.next()
                        pm_, pm_r = pm_sb.next()
                        pev = pe_[:].rearrange("p (h kt q) -> p h kt q", h=4, kt=2)
                        pmv = pm_[:].rearrange("p (h kt q) -> p h kt q", h=4, kt=2)
                        amv = amask[:].rearrange("p (h kt q) -> p h kt q", h=4, kt=2)
                        if c > 0:
                            ACT(pe_[:, 0:512], p_S[:, 0:512], AF.Exp, ["p_S"], [pe_r], scale=0.125)
                            ACT(pe_[:, 512:1024], p_S[:, 512:1024], AF.Exp, ["p_S", pe_r], [pe_r], scale=0.125)
                            TT("dve", pm_[:], pe_[:], amask[:], ALU.mult, [pe_r, "amask"], [pm_r])
                        else:
                            ACT(pev[:, 0:2, 1, :], pSv[:, 0:2, 1, :], AF.Exp, ["p_S"], [pe_r], scale=0.125)
                            ACT(pev[:, 2:4, 1, :], pSv[:, 2:4, 1, :], AF.Exp, ["p_S", pe_r], [pe_r], scale=0.125)
                            TT("dve", pmv[:, :, 1, :], pev[:, :, 1, :], amv[:, :, 1, :], ALU.mult, [pe_r, "amask"], [pm_r])
                        if bis and bis <= 1.5:
                            continue
                        for h in range(4):
                            for i_k, kt_n in enumerate(kts):
                                kti = 1 if kt_n == n else 0
                                MM(p_O[:, h, :], pmv[:, 2 * (h % 2) + h // 2, kti, :], V_g[:, kt_n, h, :], i_k == 0, i_k == len(kts) - 1,
                                   [pm_r, "V_g%d" % kt_n, "V_ones"], ["p_O"])
                        nd_, nd_r = ndt.next()
                        CP("dve", nd_[:], p_O_full[:, 0:260], ["p_O"], [nd_r])
                        DMA("sp", nd_scr[g][rows, :], nd_[:], [nd_r], ["nd_scr"])
                P.barrier()
                P.flush()
        if debug_stop == 1 or bis:
            return nc, dict(nd_scr=nd_scr)

        NTOK = S + NS
        s2_scr = dscr("s2_scr", [2, 128, NTOK], BF16)
        o_scr = dscr("o_scr", [2, 128, NTOK], BF16)
        NABI = 13
        with ExitStack() as s2a:
            tabC = sb(s2a, "tabC", [128, 8, 256], F32)
            tabS = sb(s2a, "tabS", [128, 8, 256], F32)
            w_glu_b = sb(s2a, "w_glu_b", [128, 2, 256], BF16)
            DMA("pool", w_glu_b[:], w_glu.rearrange("(ck p) n -> p ck n", p=128), (), ["w_glu_b"])
            R = ["par"]
            TS("dve", pr(NABI), pr(ABI), -1.0, None, ALU.mult, None, R, R)
            with ExitStack() as stt:
                tt = [sb(stt, "tt%d" % i, [128, 8, 128], F32) for i in range(4)]
                CP("dve", tabC[:, :, 0:1], pr(COS).unsqueeze(2), R, ["tab"])
                CP("dve", tabS[:, :, 0:1], pr(SIN).unsqueeze(2), R, ["tab"])
                m = 1
                while m < 256:
                    cm = tabC[:, :, m - 1:m].to_broadcast([128, 8, m])
                    sm = tabS[:, :, m - 1:m].to_broadcast([128, 8, m])
                    c0 = tabC[:, :, 0:m]; s0 = tabS[:, :, 0:m]
                    t = [tt[i][:, :, 0:m] for i in range(4)]
                    TT("dve", t[0], c0, cm, ALU.mult, ["tab"], ["tt0"])
                    TT("dve", t[1], s0, sm, ALU.mult, ["tab"], ["tt1"])
                    TT("dve", t[2], s0, cm, ALU.mult, ["tab"], ["tt2"])
                    TT("dve", t[3], c0, sm, ALU.mult, ["tab"], ["tt3"])
                    TT("dve", tabC[:, :, m:2 * m], t[0], t[1], ALU.subtract, ["tt0", "tt1", "tab"], ["tab"])
                    TT("dve", tabS[:, :, m:2 * m], t[2], t[3], ALU.add, ["tt2", "tt3", "tab"], ["tab"])
                    m *= 2
                P.barrier()
                P.flush()
            p_bu = psum(s2a, "p_bu", [128, 2, 512], F32)
            p_y = psum(s2a, "p_y", [128, 2, 512], F32)
            p_z = psum(s2a, "p_z", [128, 2, 512], F32)
            p_tr = psum(s2a, "p_tr", [128, 2, 512], BF16)
            p_h0 = psum(s2a, "p_h0", [128, 512], F32)
            tm = [sb(s2a, "tm%d" % i, [128, 256], F32) for i in range(4)]
            bt = [sb(s2a, "bt%d" % i, [128, 256], F32) for i in range(2)]
            ht = [sb(s2a, "ht%d" % i, [128, 256], F32) for i in range(2)]
            Hb = [sb(s2a, "Hb%d" % i, [128, 512], BF16) for i in range(2)]
            s_f = sb(s2a, "s_f", [128, 2, 512], F32)
            s_b = sb(s2a, "s_b", [128, 2, 512], BF16)
            g1 = sb(s2a, "g1", [128, 2, 512], F32)
            g2 = sb(s2a, "g2", [128, 2, 512], F32)
            s2T = sb(s2a, "s2T", [128, 2, 512], BF16)
            ndl = Rot("ndl", [sb(s2a, "ndl%d" % i, [128, 3, 260], F32) for i in range(2)])
            rdn = sb(s2a, "rdn", [128, 4], F32)
            o_b = Rot("o_b", [sb(s2a, "o_b%d" % i, [128, 256], BF16) for i in range(2)])
            oT_g = sb(s2a, "oT_g", [128, 2, 512], BF16)
            h0s = sb(s2a, "h0s", [NS, 1024, 2], F32)
            h0T = sb(s2a, "h0T", [128, 8, 2, NS], F32)
            hsn = sb(s2a, "hsn", [128, 2, NS], F32)
            hs_out = sb(s2a, "hs_out", [NS, 1024, 2], F32)

            def gelu_glu(ntok, col0):
                for ck in range(2):
                    y = p_y[:, ck, 0:ntok]
                    sf = s_f[:, ck, 0:ntok]; a1 = g1[:, ck, 0:ntok]; a2 = g2[:, ck, 0:ntok]
                    ACT(a1, y, AF.Square, ["p_y"], ["g1"])
                    TS("dve", a1, a1, 0.044715, 1.0, ALU.mult, ALU.add, ["g1"], ["g1"])
                    TT("dve", a1, a1, y, ALU.mult, ["g1", "p_y"], ["g1"])
                    ACT(a2, a1, AF.Sigmoid, ["g1"], ["g2"], scale=1.5957691216057308)
                    TT("dve", sf, a2, y, ALU.mult, ["g2", "p_y"], ["s_f"])
                    CP("act", s_b[:, ck, 0:ntok], sf, ["s_f"], ["s_b"])
                for co in range(2):
                    for ck in range(2):
                        MM(p_z[:, co, 0:ntok], w_glu_b[:, ck, 128 * co:128 * co + 128], s_b[:, ck, 0:ntok], ck == 0, ck == 1,
                           ["w_glu_b", "s_b"], ["p_z"])
                    ACT(g2[:, co, 0:ntok], p_z[:, co, 0:ntok], AF.Sigmoid, ["p_z", "g2"], ["g2"], bias=vecs[:, 2 + co:3 + co])
                TT("pool", s2T[:, :, 0:ntok], s_f[:, :, 0:ntok], g2[:, :, 0:ntok], ALU.mult, ["s_f", "g2"], ["s2T"])
                for ck in range(2):
                    DMA("sp", s2_scr[ck, :, col0:col0 + ntok], s2T[:, ck, 0:ntok], ["s2T"], ["s2_scr"])

            for G in range(8):
                cols = slice(512 * G, 512 * G + 512)
                for ck in range(2):
                    MM(p_y[:, ck, :], diagD[:, ck, :], uT[:, ck, cols], True, False, ["diagD", "uT_all"], ["p_y"])
                    for st_ in range(4 * ck, 4 * ck + 4):
                        MM(p_bu[:, 0, :], bbT[0][:, st_, :], uT[:, ck, cols], True, True, ["bbT0", "uT_all"], ["p_bu"])
                        MM(p_bu[:, 1, :], bbT[1][:, st_, :], uT[:, ck, cols], True, True, ["bbT1", "uT_all"], ["p_bu"])
                        rho_b = par[:, RHO, st_:st_ + 1].to_broadcast([128, 256])
                        cr = "carry%d" % st_
                        for hf in range(2):
                            c0 = 256 * hf
                            b_r = p_bu[:, 0, c0:c0 + 256]; b_i = p_bu[:, 1, c0:c0 + 256]
                            Cc = tabC[:, st_, :]; Sn = tabS[:, st_, :]
                            TT("dve", tm[0][:], b_r, Cc, ALU.mult, ["p_bu", "tab"], ["tm0"])
                            TT("dve", tm[1][:], b_i, Sn, ALU.mult, ["p_bu", "tab"], ["tm1"])
                            TT("dve", tm[2][:], b_i, Cc, ALU.mult, ["p_bu", "tab"], ["tm2"])
                            TT("dve", tm[3][:], b_r, Sn, ALU.mult, ["p_bu", "tab"], ["tm3"])
                            TT("pool", bt[0][:], tm[0][:], tm[1][:], ALU.add, ["tm0", "tm1"], ["bt0"])
                            TT("pool", bt[1][:], tm[2][:], tm[3][:], ALU.subtract, ["tm2", "tm3"], ["bt1"])
                            P.op("dve", lambda e, st_=st_, rho_b=rho_b: e.tensor_tensor_scan(
                                out=ht[0][:], data0=rho_b, data1=bt[0][:], initial=carry[:, st_, 0:1], op0=ALU.mult, op1=ALU.add),
                                ["bt0", cr, "par"], ["ht0"])
                            P.op("dve", lambda e, st_=st_, rho_b=rho_b: e.tensor_tensor_scan(
                                out=ht[1][:], data0=rho_b, data1=bt[1][:], initial=carry[:, st_, 1:2], op0=ALU.mult, op1=ALU.add),
                                ["bt1", cr, "par"], ["ht1"])
                            TT("dve", tm[0][:], ht[0][:], Cc, ALU.mult, ["ht0", "tab"], ["tm0"])
                            TT("pool", tm[1][:], ht[1][:], Sn, ALU.mult, ["ht1", "tab"], ["tm1"])
                            TT("dve", tm[2][:], ht[1][:], Cc, ALU.mult, ["ht1", "tab"], ["tm2"])
                            TT("pool", tm[3][:], ht[0][:], Sn, ALU.mult, ["ht0", "tab"], ["tm3"])
                            TT("pool", Hb[0][:, c0:c0 + 256], tm[0][:], tm[1][:], ALU.subtract, ["tm0", "tm1"], ["Hb0"])
                            TT("pool", Hb[1][:, c0:c0 + 256], tm[2][:], tm[3][:], ALU.add, ["tm2", "tm3"], ["Hb1"])
                            TT("pool", carry[:, st_, 0:1], tm[0][:, 255:256], tm[1][:, 255:256], ALU.subtract, ["tm0", "tm1", cr], [cr])
                            TT("pool", carry[:, st_, 1:2], tm[2][:, 255:256], tm[3][:, 255:256], ALU.add, ["tm2", "tm3", cr], [cr])
                        last = (st_ == 4 * ck + 3)
                        MM(p_y[:, ck, :], cT[0][:, st_, :], Hb[0][:], False, False, ["cT0", "Hb0"], ["p_y"])
                        MM(p_y[:, ck, :], cT[1][:, st_, :], Hb[1][:], False, last, ["cT1", "Hb1"], ["p_y"])
                gelu_glu(512, 512 * G)
                for ti in range(4):
                    n = 4 * G + ti
                    nl, nl_r = ndl.next()
                    for g in range(3):
                        DMA("sp", nl[:, g, :], nd_scr[g][128 * n:128 * n + 128, :], [nl_r], [nl_r])
                    TT("pool", nl[:, 0, :], nl[:, 0, :], nl[:, 1, :], ALU.add, [nl_r], [nl_r])
                    TT("pool", nl[:, 0, :], nl[:, 0, :], nl[:, 2, :], ALU.add, [nl_r], [nl_r])
                    nv = nl[:, 0, :].rearrange("p (h d) -> p h d", h=4)
                    P.op("dve", lambda e, nv=nv: e.reciprocal(out=rdn[:], in_=nv[:, :, 64]), [nl_r], ["rdn"])
                    ob, ob_r = o_b.next()
                    TT("dve", ob[:].rearrange("p (h d) -> p h d", h=4), nv[:, :, 0:64], rdn[:].unsqueeze(2).to_broadcast([128, 4, 64]),
                       ALU.mult, [nl_r, "rdn"], [ob_r])
                    for j in range(2):
                        TR(p_tr[:, j, 128 * ti:128 * ti + 128], ob[:, 128 * j:128 * j + 128], ident_b[:], [ob_r, "ident_b"], ["p_tr"])
                CP("dve", oT_g[:], p_tr[:], ["p_tr"], ["oT_g"])
                for j in range(2):
                    DMA("sp", o_scr[j, :, 512 * G:512 * G + 512], oT_g[:, j, :], ["oT_g"], ["o_scr"])
            DMA("sp", ssm_p.rearrange("(st q) two -> q st two", q=128), carry[:], ["carry%d" % i for i in range(8)], ["ssm_p"])

            DMA("sp", h0s[:], st_ssm.rearrange("s (n two) -> s n two", two=2), (), ["h0s"])
            for st_ in range(8):
                for ri in range(2):
                    TR(p_h0[:, (2 * st_ + ri) * NS:(2 * st_ + ri + 1) * NS], h0s[:, 128 * st_:128 * st_ + 128, ri], ident_f[0:NS, 0:NS],
                       ["h0s", "ident_f"], ["p_h0"])
            CP("dve", h0T[:].rearrange("p a b c -> p (a b c)"), p_h0[:, 0:16 * NS], ["p_h0"], ["h0T"])
            for ck in range(2):
                MM(p_y[:, ck, 0:NS], diagD[:, ck, :], uT_s[:, ck, :], True, False, ["diagD", "uT_s"], ["p_y"])
                for st_ in range(4 * ck, 4 * ck + 4):
                    MM(p_bu[:, 0, 0:NS], bbT[0][:, st_, :], uT_s[:, ck, :], True, True, ["bbT0", "uT_s"], ["p_bu"])
                    MM(p_bu[:, 1, 0:NS], bbT[1][:, st_, :], uT_s[:, ck, :], True, True, ["bbT1", "uT_s"], ["p_bu"])
                    abr = par[:, ABR, st_:st_ + 1]; abi = par[:, ABI, st_:st_ + 1]; nabi = par[:, NABI, st_:st_ + 1]
                    h0r = h0T[:, st_, 0, :]; h0i = h0T[:, st_, 1, :]
                    TS("dve", hsn[:, 0, :], h0r, abr, None, ALU.mult, None, ["h0T", "par"], ["hsn"])
                    STT("dve", hsn[:, 0, :], h0i, nabi, hsn[:, 0, :], ALU.mult, ALU.add, ["h0T", "par", "hsn"], ["hsn"])
                    TT("dve", hsn[:, 0, :], hsn[:, 0, :], p_bu[:, 0, 0:NS], ALU.add, ["hsn", "p_bu"], ["hsn"])
                    TS("dve", hsn[:, 1, :], h0i, abr, None, ALU.mult, None, ["h0T", "par", "hsn"], ["hsn"])
                    STT("dve", hsn[:, 1, :], h0r, abi, hsn[:, 1, :], ALU.mult, ALU.add, ["h0T", "par", "hsn"], ["hsn"])
                    TT("dve", hsn[:, 1, :], hsn[:, 1, :], p_bu[:, 1, 0:NS], ALU.add, ["hsn", "p_bu"], ["hsn"])
                    CP("act", Hb[0][:, 0:NS], hsn[:, 0, :], ["hsn"], ["Hb0"])
                    CP("act", Hb[1][:, 0:NS], hsn[:, 1, :], ["hsn"], ["Hb1"])
                    last = (st_ == 4 * ck + 3)
                    MM(p_y[:, ck, 0:NS], cT[0][:, st_, :], Hb[0][:, 0:NS], False, False, ["cT0", "Hb0"], ["p_y"])
                    MM(p_y[:, ck, 0:NS], cT[1][:, st_, :], Hb[1][:, 0:NS], False, last, ["cT1", "Hb1"], ["p_y"])
                    for ri in range(2):
                        TR(p_h0[0:NS, 256:384], hsn[:, ri, :], ident_f[:], ["hsn", "ident_f"], ["p_h0"])
                        CP("dve", hs_out[:, 128 * st_:128 * st_ + 128, ri], p_h0[0:NS, 256:384], ["p_h0"], ["hs_out"])
            DMA("sp", ssm_s, hs_out[:].rearrange("s n two -> s (n two)"), ["hs_out"], ["ssm_s"])
            gelu_glu(NS, S)
            for j in range(2):
                DMA("sp", o_scr[j, :, S:S + NS], attn_oT_s[:, j, :], ["attn_oT_s"], ["o_scr"])
            if debug_stop == 2:
                dbg_s2 = dout("dbg_s2", [2, 128, NTOK], BF16)
                dbg_o = dout("dbg_o", [2, 128, NTOK], BF16)
                DMA("sp", dbg_s2, s2_scr, ["s2_scr"], ["dbg_s2"])
                DMA("sp", dbg_o, o_scr, ["o_scr"], ["dbg_o"])
            P.barrier()
            P.flush()
        if debug_stop == 2:
            return nc, {}
        stB.close()

        def layer_norm_rows(rows, h, g_t, b_t, stats, mv, rstd, hres):
            for half in range(2):
                P.op("dve", lambda e, half=half: e.bn_stats(out=stats[:rows, half, :], in_=h[:rows, 512 * half:512 * half + 512]), [hres], ["ln_stats"])
            P.op("dve", lambda e: e.bn_aggr(out=mv[:rows, :], in_=stats[:rows, :, :].rearrange("p a b -> p (a b)")), ["ln_stats"], ["ln_mv"])
            ACT(rstd[:rows, :], mv[:rows, 1:2], AF.Sqrt, ["ln_mv", "eps"], ["ln_rstd"], bias=eps_t[:rows, :])
            P.op("dve", lambda e: e.reciprocal(out=rstd[:rows, :], in_=rstd[:rows, :]), ["ln_rstd"], ["ln_rstd"])
            TS("dve", h[:rows, :], h[:rows, :], mv[:rows, 0:1], rstd[:rows, 0:1], ALU.subtract, ALU.mult, [hres, "ln_mv", "ln_rstd"], [hres])
            TT("pool", h[:rows, :], h[:rows, :], g_t[:rows, :], ALU.mult, [hres, "ln_g"], [hres])
            TT("pool", h[:rows, :], h[:rows, :], b_t[:rows, :], ALU.add, [hres, "ln_b"], [hres])

        with ExitStack() as s2b:
            ln1g_t = sb(s2b, "ln1g_t", [128, D], F32); ln1b_t = sb(s2b, "ln1b_t", [128, D], F32)
            DMA("sp", ln1g_t[:], ln1_g.partition_broadcast(128), (), ["ln_g"])
            DMA("sp", ln1b_t[:], ln1_b.partition_broadcast(128), (), ["ln_b"])
            w_gate_b = sb(s2b, "w_gate_b", [128, 8, 2048], BF16)
            w_out_b = sb(s2b, "w_out_b", [128, 8, 1024], BF16)
            w_attn_b = sb(s2b, "w_attn_b", [128, 2, 1024], BF16)
            w_ssmbr_b = sb(s2b, "w_ssmbr_b", [128, 2, 1024], BF16)
            wgv = w_gate.rearrange("(kc p) n -> p kc n", p=128)
            for j in range(4):
                DMA("pool", w_gate_b[:, :, 512 * j:512 * j + 512], wgv[:, :, 512 * j:512 * j + 512], (), ["w_gate_b"])
            wov = w_out.rearrange("(kc p) n -> p kc n", p=128)
            for j in range(2):
                DMA("pool", w_out_b[:, :, 512 * j:512 * j + 512], wov[:, :, 512 * j:512 * j + 512], (), ["w_out_b"])
            DMA("pool", w_attn_b[:], w_attn_br.rearrange("(kc p) n -> p kc n", p=128), (), ["w_attn_b"])
            DMA("pool", w_ssmbr_b[:], w_ssm_br.rearrange("(kc p) n -> p kc n", p=128), (), ["w_ssmbr_b"])
            zt = sb(s2b, "zt", [128, 2048], BF16)
            MSET("pool", zt[:], 0.0, ["zt"])
            xgz = xg_scr.rearrange("(p a two) d -> p a (two d)", p=128, two=2)
            for j in range(8):
                DMA("sp", xgz[:, 32 * j:32 * j + 32, :], zt[:].unsqueeze(1).to_broadcast([128, 32, 2048]), ["zt"], ["xg_zero"])
            p_xT = psum(s2b, "p_xT2", [128, 8, 128], BF16)
            pA = psum(s2b, "pA", [128, 512], F32); pS_ = psum(s2b, "pS_", [128, 512], F32)
            pGA = psum(s2b, "pGA", [128, 512], F32); pGS = psum(s2b, "pGS", [128, 512], F32)
            pM = psum(s2b, "pM", [128, 2, 512], F32)
            x_f4 = sb(s2b, "x_f4", [128, 4, D], F32)
            x_b2 = Rot("x_b2", [sb(s2b, "x_b2_%d" % i, [128, D], BF16) for i in range(2)])
            xT_g = sb(s2b, "xT_g", [128, 8, 512], BF16)
            s2l = sb(s2b, "s2l", [128, 2, 512], BF16); oTl = sb(s2b, "oTl", [128, 2, 512], BF16)
            gA = sb(s2b, "gA", [128, 512], F32); gS = sb(s2b, "gS", [128, 512], F32)
            m1 = sb(s2b, "m1", [128, 512], F32); m2 = sb(s2b, "m2", [128, 512], F32)
            mergedT = sb(s2b, "mergedT", [128, 8, 512], BF16)
            h_t = Rot("h_t", [sb(s2b, "h_t%d" % i, [128, D], F32) for i in range(2)])
            stats = sb(s2b, "stats", [128, 2, 6], F32); mv = sb(s2b, "mv", [128, 2], F32); rstd = sb(s2b, "rstd", [128, 1], F32)
            for G in range(9):
                smp = (G == 8)
                ntok = NS if smp else 512
                tiles = [(0, NS)] if smp else [(ti, 128) for ti in range(4)]
                col0 = S if smp else 512 * G
                for ti, rows in tiles:
                    src = x_s if smp else x_p[512 * G + 128 * ti:512 * G + 128 * ti + 128, :]
                    DMA("sp", x_f4[:rows, ti, :], src, ["x_f4_%d" % ti], ["x_f4_%d" % ti])
                    xb, xb_r = x_b2.next()
                    CP("act", xb[:rows, :], x_f4[:rows, ti, :], ["x_f4_%d" % ti], [xb_r])
                    for kc in range(8):
                        TR(p_xT[:, kc, 0:rows], xb[:rows, kc * 128:(kc + 1) * 128], ident_b[0:rows, 0:rows], [xb_r, "ident_b"], ["p_xT2"])
                    CP("dve", xT_g[:, :, 128 * ti:128 * ti + rows], p_xT[:, :, 0:rows], ["p_xT2"], ["xT_g"])
                for j in range(2):
                    DMA("sp", s2l[:, j, 0:ntok], s2_scr[j, :, col0:col0 + ntok], ["s2l"], ["s2l"])
                    DMA("sp", oTl[:, j, 0:ntok], o_scr[j, :, col0:col0 + ntok], ["oTl"], ["oTl"])
                for dc in range(8):
                    dsl = slice(128 * dc, 128 * dc + 128)
                    for j in range(2):
                        MM(pA[:, 0:ntok], w_attn_b[:, j, dsl], oTl[:, j, 0:ntok], j == 0, j == 1, ["w_attn_b", "oTl"], ["pA"])
                    for j in range(2):
                        MM(pS_[:, 0:ntok], w_ssmbr_b[:, j, dsl], s2l[:, j, 0:ntok], j == 0, j == 1, ["w_ssmbr_b", "s2l"], ["pS_"])
                    for kc in range(8):
                        MM(pGA[:, 0:ntok], w_gate_b[:, kc, dsl], xT_g[:, kc, 0:ntok], kc == 0, kc == 7, ["w_gate_b", "xT_g"], ["pGA"])
                    for kc in range(8):
                        MM(pGS[:, 0:ntok], w_gate_b[:, kc, 1024 + 128 * dc:1024 + 128 * dc + 128], xT_g[:, kc, 0:ntok], kc == 0, kc == 7,
                           ["w_gate_b", "xT_g"], ["pGS"])
                    ACT(gA[:, 0:ntok], pGA[:, 0:ntok], AF.Sigmoid, ["pGA"], ["gA"], bias=vecs[:, 4 + dc:5 + dc])
                    ACT(gS[:, 0:ntok], pGS[:, 0:ntok], AF.Sigmoid, ["pGS"], ["gS"], bias=vecs[:, 12 + dc:13 + dc])
                    TT("dve", m1[:, 0:ntok], gA[:, 0:ntok], pA[:, 0:ntok], ALU.mult, ["gA", "pA"], ["m1"])
                    TT("dve", m2[:, 0:ntok], gS[:, 0:ntok], pS_[:, 0:ntok], ALU.mult, ["gS", "pS_"], ["m2"])
                    TT("pool", mergedT[:, dc, 0:ntok], m1[:, 0:ntok], m2[:, 0:ntok], ALU.add, ["m1", "m2"], ["mergedT"])
                for ti, rows in tiles:
                    for half in range(2):
                        for kc in range(8):
                            MM(pM[:rows, half, :], mergedT[:, kc, 128 * ti:128 * ti + rows], w_out_b[:, kc, 512 * half:512 * half + 512],
                               kc == 0, kc == 7, ["mergedT", "w_out_b"], ["pM"])
                    ht, ht_r = h_t.next()
                    for half in range(2):
                        hs = slice(512 * half, 512 * half + 512)
                        STT("dve", ht[:rows, hs], x_f4[:rows, ti, hs], ALPHA, pM[:rows, half, :], ALU.mult, ALU.add,
                            ["x_f4_%d" % ti, "pM", ht_r], [ht_r])
                    layer_norm_rows(rows, ht, ln1g_t, ln1b_t, stats, mv, rstd, ht_r)
                    r0 = col0 + 128 * ti
                    DMA("sp", x1_scr[r0:r0 + rows, :], ht[:rows, :], [ht_r], ["x1_scr"])
            if debug_stop == 2.5:
                dbg_x1 = dout("dbg_x1", [NTOK, D])
                DMA("sp", dbg_x1, x1_scr, ["x1_scr"], ["dbg_x1"])
            P.barrier()
            P.flush()
        if debug_stop == 2.5:
            return nc, {}

        with ExitStack() as s3:
            w_router_b = sb(s3, "w_router_b", [128, 8, NEXP], BF16)
            ws1_b = sb(s3, "ws1_b", [128, 8, 256], BF16); ws3_b = sb(s3, "ws3_b", [128, 8, 256], BF16)
            ws2_b = sb(s3, "ws2_b", [128, 2, D], BF16)
            w_pleg_b = sb(s3, "w_pleg_b", [128, 8, D], BF16); w_ple_b = sb(s3, "w_ple_b", [128, 2, D], BF16)
            DMA("pool", w_router_b[:], w_router.rearrange("(kc p) n -> p kc n", p=128), (), ["w_router_b"])
            DMA("pool", ws1_b[:], ws1.rearrange("(kc p) n -> p kc n", p=128), (), ["ws1_b"])
            DMA("pool", ws3_b[:], ws3.rearrange("(kc p) n -> p kc n", p=128), (), ["ws3_b"])
            DMA("pool", ws2_b[:], ws2.rearrange("(kc p) n -> p kc n", p=128), (), ["ws2_b"])
            wpv = w_ple_gate.rearrange("(kc p) n -> p kc n", p=128)
            for j in range(2):
                DMA("pool", w_pleg_b[:, :, 512 * j:512 * j + 512], wpv[:, :, 512 * j:512 * j + 512], (), ["w_pleg_b"])
            DMA("pool", w_ple_b[:], w_ple.rearrange("(kc p) n -> p kc n", p=128), (), ["w_ple_b"])
            cf = sb(s3, "cf", [128, 128], F32)
            utri_b = sb(s3, "utri_b", [128, 128], BF16); ones_b = sb(s3, "ones_b", [128, 128], BF16)
            iota_t = sb(s3, "iota_t", [128, NEXP], F32); base_t = sb(s3, "base_t", [128, NEXP], F32)
            DMA("sp", cf[:], c_utri, (), ["cf"])
            CP("dve", utri_b[:], cf[:], ["cf"], ["utri_b"])
            MSET("dve", ones_b[:], 1.0, ["ones_b"])
            DMA("sp", iota_t[:], c_iota, (), ["iota_t"])
            MSET("dve", base_t[:], 0.0, ["base_t"])
            p_T = psum(s3, "p_T3", [128, 8, 128], BF16)
            p_pT = psum(s3, "p_pT", [128, 8, 128], BF16)
            p_R = psum(s3, "p_R", [128, 512], F32)
            p_H = psum(s3, "p_H", [128, 4, 128], F32)
            p_A = psum(s3, "p_A", [128, 2, 512], F32)
            p_B = psum(s3, "p_B", [128, 2, 512], F32)
            x1f = Rot("x1f", [sb(s3, "x1f%d" % i, [128, D], F32) for i in range(2)])
            x1b = Rot("x1b", [sb(s3, "x1b%d" % i, [128, D], BF16) for i in range(2)])
            x1T = Rot("x1T", [sb(s3, "x1T%d" % i, [128, 8, 128], BF16) for i in range(2)])
            pf = Rot("pf", [sb(s3, "pf%d" % i, [128, 256], F32) for i in range(2)])
            pb = sb(s3, "pb", [128, 256], BF16); pT = sb(s3, "pT", [128, 2, 128], BF16)
            sc = sb(s3, "sc", [128, NEXP], F32); bia = sb(s3, "bia", [128, NEXP], F32)
            top8 = sb(s3, "top8", [128, 8], F32); idx8 = sb(s3, "idx8", [128, 8], U32); e8f = sb(s3, "e8f", [128, 8], F32)
            mask = sb(s3, "mask", [128, NEXP], F32); mask_b = sb(s3, "mask_b", [128, NEXP], BF16)
            ssel = sb(s3, "ssel", [128, NEXP], F32); den = sb(s3, "den", [128, 1], F32); gd = sb(s3, "gd", [128, NEXP], F32)
            rankp = sb(s3, "rankp", [128, NEXP], F32); junk = sb(s3, "junk", [128, NEXP], F32)
            rank8 = sb(s3, "rank8", [128, 8], F32); gate8 = sb(s3, "gate8", [128, 8], F32)
            destf = sb(s3, "destf", [128, 8], F32); ov = sb(s3, "ov", [128, 8], F32)
            hsf = sb(s3, "hsf", [128, 2, 128], F32); hsT = sb(s3, "hsT", [128, 2, 128], BF16)
            sg = sb(s3, "sg", [128, D], F32)
            Rt = Rot("Rt", [sb(s3, "Rt%d" % i, [128, D], F32) for i in range(2)])
            bc3 = {}
            P.ops["pool"].append(lambda e: bc3.__setitem__("r", e.to_reg(NEXP * CAP - 1)))
            for i in range(NT + 1):
                smp = (i == NT)
                rows = NS if smp else 128
                r0 = S if smp else 128 * i
                xf, xf_r = x1f.next(); xb, xb_r = x1b.next(); xT, xT_r = x1T.next(); pf_, pf_r = pf.next()
                DMA("sp", xf[:rows, :], x1_scr[r0:r0 + rows, :], ["x1_scr", xf_r], [xf_r])
                DMA("sp", pf_[:rows, :], (p_s if smp else p_p[r0:r0 + rows, :]), [pf_r], [pf_r])
                CP("act", xb[:rows, :], xf[:rows, :], [xf_r], [xb_r])
                for kc in range(8):
                    TR(p_T[:, kc, 0:rows], xb[:rows, kc * 128:(kc + 1) * 128], ident_b[0:rows, 0:rows], [xb_r, "ident_b"], ["p_T3"])
                CP("dve", xT[:, :, 0:rows], p_T[:, :, 0:rows], ["p_T3"], [xT_r])
                CP("act", pb[:rows, :], pf_[:rows, :], [pf_r], ["pb"])
                for j in range(2):
                    TR(p_pT[:, j, 0:rows], pb[:rows, j * 128:(j + 1) * 128], ident_b[0:rows, 0:rows], ["pb", "ident_b"], ["p_pT"])
                CP("dve", pT[:, :, 0:rows], p_pT[:, 0:2, 0:rows], ["p_pT"], ["pT"])
                for kc in range(8):
                    MM(p_R[:rows, 0:64], xT[:, kc, 0:rows], w_router_b[:, kc, :], kc == 0, kc == 7, [xT_r, "w_router_b"], ["p_R"])
                ACT(sc[:rows, :], p_R[:rows, 0:64], AF.Sigmoid, ["p_R"], ["sc"])
                TT("dve", bia[:rows, :], sc[:rows, :], rbias_t[:rows, :], ALU.add, ["sc", "rbias"], ["bia"])
                P.op("dve", lambda e, rows=rows: e.max(out=top8[:rows, :], in_=bia[:rows, :]), ["bia"], ["top8"])
                P.op("dve", lambda e, rows=rows: e.max_index(out=idx8[:rows, :], in_max=top8[:rows, :], in_values=bia[:rows, :]),
                     ["bia", "top8"], ["idx8"])
                CP("dve", e8f[:rows, :], idx8[:rows, :], ["idx8"], ["e8f"])
                TS("dve", mask[:rows, :], bia[:rows, :], top8[:rows, 7:8], None, ALU.is_ge, None, ["bia", "top8"], ["mask"])
                CP("dve", mask_b[:rows, :], mask[:rows, :], ["mask"], ["mask_b"])
                TT("dve", ssel[:rows, :], sc[:rows, :], mask[:rows, :], ALU.mult, ["sc", "mask"], ["ssel"])
                P.op("dve", lambda e, rows=rows: e.tensor_reduce(out=den[:rows, :], in_=ssel[:rows, :], axis=AX.X, op=ALU.add), ["ssel"], ["den"])
                P.op("dve", lambda e, rows=rows: e.reciprocal(out=den[:rows, :], in_=den[:rows, :]), ["den"], ["den"])
                TS("dve", gd[:rows, :], ssel[:rows, :], den[:rows, 0:1], 2.5, ALU.mult, ALU.mult, ["ssel", "den"], ["gd"])
                MM(p_R[:rows, 64:128], utri_b[:rows, :rows], mask_b[:rows, :], True, True, ["utri_b", "mask_b"], ["p_R"])
                TT("dve", rankp[:rows, :], p_R[:rows, 64:128], base_t[:rows, :], ALU.add, ["p_R", "base_t"], ["rankp"])
                if not smp:
                    MM(p_R[:, 128:192], ones_b[:, :], mask_b[:, :], True, True, ["ones_b", "mask_b"], ["p_R"])
                    TT("dve", base_t[:], base_t[:], p_R[:, 128:192], ALU.add, ["p_R", "base_t"], ["base_t"])
                for k in range(8):
                    P.op("dve", lambda e, rows=rows, k=k: e.scalar_tensor_tensor(
                        out=junk[:rows, :], in0=iota_t[:rows, :], scalar=e8f[:rows, k:k + 1], in1=rankp[:rows, :],
                        op0=ALU.is_equal, op1=ALU.mult, accum_out=rank8[:rows, k:k + 1]), ["iota_t", "e8f", "rankp", "junk"], ["junk", "rank8"])
                    P.op("dve", lambda e, rows=rows, k=k: e.scalar_tensor_tensor(
                        out=junk[:rows, :], in0=iota_t[:rows, :], scalar=e8f[:rows, k:k + 1], in1=gd[:rows, :],
                        op0=ALU.is_equal, op1=ALU.mult, accum_out=gate8[:rows, k:k + 1]), ["iota_t", "e8f", "gd", "junk"], ["junk", "gate8"])
                STT("dve", destf[:rows, :], e8f[:rows, :], float(CAP), rank8[:rows, :], ALU.mult, ALU.add, ["e8f", "rank8"], ["destf"])
                P.op("dve", lambda e, rows=rows: e.tensor_single_scalar(out=ov[:rows, :], in_=rank8[:rows, :], scalar=float(CAP) - 0.5, op=ALU.is_gt),
                     ["rank8"], ["ov"])
                STT("dve", destf[:rows, :], ov[:rows, :], 1.0e6, destf[:rows, :], ALU.mult, ALU.add, ["ov", "destf"], ["destf"])
                TS("dve", ov[:rows, :], ov[:rows, :], -1.0, 1.0, ALU.mult, ALU.add, ["ov"], ["ov"])
                TT("dve", gate_t[:rows, i, :], gate8[:rows, :], ov[:rows, :], ALU.mult, ["gate8", "ov"], ["gate_t"])
                CP("dve", dest_t[:rows, i, :], destf[:rows, :], ["destf"], ["dest_t"])
                for k in range(8):
                    P.dma("pool", lambda e, rows=rows, i=i, k=k, xb=xb: e.indirect_dma_start(
                        out=xg_scr, out_offset=bass.IndirectOffsetOnAxis(ap=dest_t[:, i, k:k + 1], axis=0),
                        in_=xb[:, :], in_offset=None, bounds_check=bc3["r"], oob_is_err=False),
                        [xb_r, "dest_t"], ["xg_scr"])
                for a_, wsx in enumerate((ws1_b, ws3_b)):
                    for ffc in range(2):
                        for kc in range(8):
                            MM(p_H[:, 2 * a_ + ffc, 0:rows], wsx[:, kc, 128 * ffc:128 * ffc + 128], xT[:, kc, 0:rows], kc == 0, kc == 7,
                               ["ws1_b", "ws3_b", xT_r], ["p_H"])
                ACT(hsf[:, :, 0:rows], p_H[:, 0:2, 0:rows], AF.Silu, ["p_H"], ["hsf"])
                TT("dve", hsT[:, :, 0:rows], hsf[:, :, 0:rows], p_H[:, 2:4, 0:rows], ALU.mult, ["hsf", "p_H"], ["hsT"])
                for half in range(2):
                    hs = slice(512 * half, 512 * half + 512)
                    for ffc in range(2):
                        MM(p_A[:rows, half, :], hsT[:, ffc, 0:rows], ws2_b[:, ffc, hs], ffc == 0, ffc == 1, ["hsT", "ws2_b"], ["p_A"])
                    for kc in range(8):
                        MM(p_B[:rows, half, :], xT[:, kc, 0:rows], w_pleg_b[:, kc, hs], kc == 0, kc == 7, [xT_r, "w_pleg_b"], ["p_B"])
                    ACT(sg[:rows, hs], p_B[:rows, half, :], AF.Sigmoid, ["p_B"], ["sg"])
                Rt_, Rt_r = Rt.next()
                for half in range(2):
                    hs = slice(512 * half, 512 * half + 512)
                    for pc in range(2):
                        MM(p_B[:rows, half, :], pT[:, pc, 0:rows], w_ple_b[:, pc, hs], pc == 0, pc == 1, ["pT", "w_ple_b", "sg"], ["p_B"])
                    STT("dve", Rt_[:rows, hs], xf[:rows, hs], ALPHA, p_A[:rows, half, :], ALU.mult, ALU.add, [xf_r, "p_A", Rt_r], [Rt_r])
                    TT("dve", sg[:rows, hs], sg[:rows, hs], p_B[:rows, half, :], ALU.mult, ["sg", "p_B"], ["sg"])
                    TT("pool", Rt_[:rows, hs], Rt_[:rows, hs], sg[:rows, hs], ALU.add, [Rt_r, "sg"], [Rt_r])
                DMA("sp", r_scr[r0:r0 + rows, :], Rt_[:rows, :], [Rt_r], ["r_scr"])
            if debug_stop == 3:
                dbg_r = dout("dbg_r", [NTOK, D])
                dbg_gate = dout("dbg_gate", [128, (NT + 1) * 8])
                dbg_dest = dout("dbg_dest", [128, (NT + 1) * 8], I32)
                DMA("sp", dbg_r, r_scr, ["r_scr"], ["dbg_r"])
                DMA("sp", dbg_gate, gate_t[:].rearrange("p a b -> p (a b)"), ["gate_t"], ["dbg_gate"])
                DMA("sp", dbg_dest, dest_t[:].rearrange("p a b -> p (a b)"), ["dest_t"], ["dbg_dest"])
            P.barrier()
            P.flush()
        if debug_stop == 3:
            return nc, {}

        with ExitStack() as s4:
            w1b = Rot("w1b", [sb(s4, "w1b%d" % i, [128, 8, 256], BF16) for i in range(2)])
            w3b = Rot("w3b", [sb(s4, "w3b%d" % i, [128, 8, 256], BF16) for i in range(2)])
            w2b = Rot("w2b", [sb(s4, "w2b%d" % i, [128, 2, D], BF16) for i in range(2)])
            xg = Rot("xg", [sb(s4, "xg%d" % i, [128, 4, D], BF16) for i in range(2)])
            xgT = sb(s4, "xgT", [128, 8, 512], BF16)
            hf = sb(s4, "hf", [128, 2, 512], F32)
            hT = sb(s4, "hT", [128, 2, 512], BF16)
            yb = Rot("yb", [sb(s4, "yb%d" % i, [128, D], F32) for i in range(3)])
            pTa = psum(s4, "pTa", [128, 4, 512], BF16)
            pH = psum(s4, "pH", [128, 4, 512], F32)
            pY = psum(s4, "pY", [128, 2, 512], F32)
            NEX = NEXP if (debug_stop is None or debug_stop >= 4) else 1
            for ex in range(NEX):
                w1_, w1_r = w1b.next(); w3_, w3_r = w3b.next(); w2_, w2_r = w2b.next()
                DMA("pool", w1_[:], w1[ex].rearrange("(kc p) n -> p kc n", p=128), [w1_r], [w1_r])
                DMA("pool", w3_[:], w3[ex].rearrange("(kc p) n -> p kc n", p=128), [w3_r], [w3_r])
                DMA("pool", w2_[:], w2[ex].rearrange("(kc p) n -> p kc n", p=128), [w2_r], [w2_r])
                for sbk in range(CAP // 512):
                    row0 = ex * CAP + 512 * sbk
                    xg_, xg_r = xg.next()
                    DMA("sp", xg_[:], xg_scr[row0:row0 + 512, :].rearrange("(b p) d -> p b d", p=128), ["xg_scr", xg_r], [xg_r])
                    for half in range(2):
                        for kcl in range(4):
                            kc = 4 * half + kcl
                            for blk in range(4):
                                TR(pTa[:, kcl, 128 * blk:128 * blk + 128], xg_[:, blk, 128 * kc:128 * kc + 128], ident_b[:],
                                   [xg_r, "ident_b"], ["pTa"])
                        for kcl in range(4):
                            CP("dve", xgT[:, 4 * half + kcl, :], pTa[:, kcl, :], ["pTa"], ["xgT%d" % (4 * half + kcl)])
                    xgT_res = ["xgT%d" % k_ for k_ in range(8)]
                    for a_, wx, wx_r in ((0, w1_, w1_r), (1, w3_, w3_r)):
                        for ffc in range(2):
                            for kc in range(8):
                                MM(pH[:, 2 * a_ + ffc, :], wx[:, kc, 128 * ffc:128 * ffc + 128], xgT[:, kc, :], kc == 0, kc == 7,
                                   [wx_r, "xgT%d" % kc], ["pH%d" % (2 * a_ + ffc)])
                    for ffc in range(2):
                        ACT(hf[:, ffc, :], pH[:, ffc, :], AF.Silu, ["pH%d" % ffc], ["hf%d" % ffc])
                        TT("dve", hT[:, ffc, :], hf[:, ffc, :], pH[:, 2 + ffc, :], ALU.mult, ["hf%d" % ffc, "pH%d" % (2 + ffc)], ["hT%d" % ffc])
                    for blk in range(4):
                        for half in range(2):
                            for ffc in range(2):
                                MM(pY[:, half, :], hT[:, ffc, 128 * blk:128 * blk + 128], w2_[:, ffc, 512 * half:512 * half + 512],
                                   ffc == 0, ffc == 1, ["hT%d" % ffc, w2_r], ["pY%d" % half])
                        yb_, yb_r = yb.next()
                        CP("act", yb_[:, 0:512], pY[:, 0, :], ["pY0", yb_r], [yb_r])
                        CP("dve", yb_[:, 512:1024], pY[:, 1, :], ["pY1", yb_r], [yb_r])
                        DMA("sp", yg_scr[row0 + 128 * blk:row0 + 128 * blk + 128, :], yb_[:], [yb_r], ["yg_scr"])
            P.barrier()
            P.flush()

        with ExitStack() as s5:
            ln2g_t = sb(s5, "ln2g_t", [128, D], F32); ln2b_t = sb(s5, "ln2b_t", [128, D], F32)
            DMA("sp", ln2g_t[:], ln2_g.partition_broadcast(128), (), ["ln_g"])
            DMA("sp", ln2b_t[:], ln2_b.partition_broadcast(128), (), ["ln_b"])
            Gb = [sb(s5, "Gb%d" % k, [128, D], F32) for k in range(8)]
            for k in range(8):
                MSET("pool" if k % 2 else "dve", Gb[k][:], 0.0, ["Gb%d" % k])
            Rt5 = Rot("Rt5", [sb(s5, "Rt5_%d" % i, [128, D], F32) for i in range(2)])
            stats = sb(s5, "stats5", [128, 2, 6], F32); mv = sb(s5, "mv5", [128, 2], F32); rstd = sb(s5, "rstd5", [128, 1], F32)
            bc5 = {}
            P.ops["pool"].append(lambda e: bc5.__setitem__("r", e.to_reg(NEXP * CAP - 1)))
            for i in range(NT + 1):
                smp = (i == NT)
                rows = NS if smp else 128
                r0 = S if smp else 128 * i
                Rt_, Rt_r = Rt5.next()
                DMA("sp", Rt_[:rows, :], r_scr[r0:r0 + rows, :], ["r_scr", Rt_r], [Rt_r])
                for k in range(8):
                    P.dma("pool", lambda e, rows=rows, i=i, k=k: e.indirect_dma_start(
                        out=Gb[k][:, :], out_offset=None, in_=yg_scr,
                        in_offset=bass.IndirectOffsetOnAxis(ap=dest_t[:, i, k:k + 1], axis=0),
                        bounds_check=bc5["r"], oob_is_err=False), ["yg_scr", "dest_t", "Gb%d" % k], ["Gb%d" % k])
                for k in range(8):
                    STT("dve", Rt_[:rows, :], Gb[k][:rows, :], gate_t[:rows, i, k:k + 1], Rt_[:rows, :], ALU.mult, ALU.add,
                        ["Gb%d" % k, "gate_t", Rt_r], [Rt_r])
                layer_norm_rows(rows, Rt_, ln2g_t, ln2b_t, stats, mv, rstd, Rt_r)
                DMA("sp", (y_s if smp else y_p[r0:r0 + rows, :]), Rt_[:rows, :], [Rt_r], ["y_out"])
            P.barrier()
            P.flush()
    return nc, {}


_NC_CACHE = {}


def _consts():
    half = 32
    inv = (np.float32(10000.0) ** (-np.arange(half, dtype=np.float32) / np.float32(half))).astype(np.float32)
    rope = np.zeros((3, S, 64), np.float32)
    for g, dil in enumerate(DILS):
        L = S // dil
        idx = np.arange(S)
        r = idx // L
        j = idx % L
        pos = (j * dil + r).astype(np.float32)
        ang = pos[:, None] * inv[None, :]
        rope[g, :, :32] = np.cos(ang)
        rope[g, :, 32:] = np.sin(ang)
    ang_s = np.float32(8192.0) * inv
    rope_s = np.concatenate([np.cos(ang_s), np.sin(ang_s)])[None, :].astype(np.float32)
    kk = np.arange(128)[:, None]
    qq = np.arange(128)[None, :]
    am = np.zeros((128, 4, 2, 128), np.float32)
    am[:, :, 0, :] = (kk >= qq)[:, None, :]
    am[:, :, 1, :] = (kk <= qq)[:, None, :]
    sel = (np.arange(128)[None, :] // 8 == np.arange(16)[:, None]).astype(np.float32)
    return {
        "c_ident": np.eye(128, dtype=np.float32),
        "c_rope": rope,
        "c_rope_s": rope_s,
        "c_amask": am.reshape(128, 1024),
        "c_utri": (kk < qq).astype(np.float32),
        "c_iota": np.tile(np.arange(64, dtype=np.float32)[None, :], (128, 1)),
        "c_sel": sel,
        "c_selT": np.ascontiguousarray(sel.T),
    }


def make_in_maps(inp, ne_in=NEXP):
    c = _consts()
    f = lambda a: np.ascontiguousarray(a, dtype=np.float32)
    shared = {
        "w_in": f(inp["w_in"][0]), "a_re": f(inp["a_re"][0]), "a_im": f(inp["a_im"][0]), "log_dt": f(inp["log_dt"][0]),
        "b_re": f(inp["b_re"][0]), "b_im": f(inp["b_im"][0]), "c_re": f(inp["c_re"][0]), "c_im": f(inp["c_im"][0]),
        "d_skip": f(inp["d_skip"][0]), "w_glu": f(inp["w_glu"][0]), "b_glu": f(inp["b_glu"][0]),
        "w_attn_br": f(inp["w_attn_br"][0]), "w_ssm_br": f(inp["w_ssm_br"][0]), "w_gate": f(inp["w_gate"][0]),
        "b_gate": f(inp["b_gate"][0]), "w_out": f(inp["w_out"][0]), "ln1_g": f(inp["ln1_g"][0]), "ln1_b": f(inp["ln1_b"][0]),
        "w_router": f(inp["w_router"][0]), "router_bias": f(inp["router_bias"][0]),
        "w1": f(inp["w1"][0, :ne_in]), "w3": f(inp["w3"][0, :ne_in]), "w2": f(inp["w2"][0, :ne_in]),
        "ws1": f(inp["ws1"][0]), "ws3": f(inp["ws3"][0]), "ws2": f(inp["ws2"][0]),
        "w_ple_gate": f(inp["w_ple_gate"][0]), "w_ple": f(inp["w_ple"][0]),
        "ln2_g": f(inp["ln2_g"][0]), "ln2_b": f(inp["ln2_b"][0]),
    }
    shared.update(c)
    maps = []
    for i in range(8):
        sl = slice(NS * i, NS * i + NS)
        m = dict(shared)
        m["x_p"] = f(inp["x_prompt"][i])
        m["x_s"] = f(inp["x_sample"][sl, 0])
        m["cache0"] = f(inp["cache_kv_w128"][0, sl]).reshape(NS, 128, 512)
        m["cache1"] = f(inp["cache_kv_w512"][0, sl]).reshape(NS, 512, 512)
        m["cache2"] = f(inp["cache_kv_w2048"][0, sl]).reshape(NS, 2048, 512)
        m["st_ssm"] = f(inp["state_ssm"][0, sl]).reshape(NS, 2048)
        m["p_p"] = f(inp["p_prompt"][0, i])
        m["p_s"] = f(inp["p_sample"][0, sl, 0])
        maps.append(m)
    return maps


def kernel(**inp):
    if "nc" not in _NC_CACHE:
        _NC_CACHE["nc"] = build()[0]
    nc = _NC_CACHE["nc"]
    maps = make_in_maps(inp)
    res = run_bass_kernel_spmd(nc, maps, core_ids=list(range(8))).results
    cat = lambda k: np.stack([r[k] for r in res], 0)
    y_p = cat("y_p").reshape(8, S, D)
    y_s = np.concatenate([r["y_s"] for r in res], 0).reshape(128, 1, D)
    outs = [y_p, y_s]
    for g in range(3):
        outs.append(cat("kv_p%d" % g).reshape(1, 8, KEEP[g], 2, 4, 64))
        outs.append(np.concatenate([r["kv_s%d" % g] for r in res], 0).reshape(1, 128, 1, 2, 4, 64))
    outs.append(cat("ssm_p").reshape(1, 8, 16, 64, 2))
    outs.append(np.concatenate([r["ssm_s"] for r in res], 0).reshape(1, 128, 16, 64, 2))
    return tuple(np.ascontiguousarray(o, dtype=np.float32) for o in outs)
```

```python
import math
from contextlib import ExitStack
import numpy as np
import concourse.bass as bass
import concourse.mybir as mybir
from concourse.bass_utils import run_bass_kernel_spmd

F32 = mybir.dt.float32
BF16 = mybir.dt.bfloat16
I32 = mybir.dt.int32
U32 = mybir.dt.uint32
AF = mybir.ActivationFunctionType
ALU = mybir.AluOpType
AX = mybir.AxisListType

ENGS = ("pe", "act", "dve", "pool", "sp")

S = 4096
NT = S // 128
NS = 16
D = 1024
CAP = 1024
NEXP = 64
ALPHA = 2.0 ** 0.25
LN_EPS = 1e-5
DILS = (1, 4, 16)
KEEP = (128, 512, 2048)
TWO_PI = 2.0 * math.pi


class Prog:
    def __init__(self, nc, stack, n_dma_sems=40):
        self.nc = nc
        self.ops = {e: [] for e in ENGS}
        self.cnt = {e: 0 for e in ENGS}
        self.sem = {e: stack.enter_context(nc.semaphore("c_" + e)) for e in ENGS}
        self.dsem = [stack.enter_context(nc.semaphore("d%d" % i)) for i in range(n_dma_sems)]
        self.dval = [0] * n_dma_sems
        self.dnext = 0
        self.seen = {e: {} for e in ENGS}
        self.res = {}
        self.ninst = 0
        self.swsem = [stack.enter_context(nc.semaphore("w%d" % i)) for i in range(8)]
        self.sw_pending = [None] * 8
        self.swval = [0] * 8
        self.sw_token = {}
        self.sw_next = 0
        self.sw_serial = 0
        self.sw_mark = stack.enter_context(nc.sbuf_tensor("sw_mark", [1, 8], F32))

    def _retire(self, j):
        return

    def _deps(self, reads, writes):
        deps = []
        for r in reads:
            st = self.res.get(r)
            if st and st["w"] is not None:
                deps.append(st["w"])
        for w in writes:
            st = self.res.get(w)
            if st:
                if st["w"] is not None:
                    deps.append(st["w"])
                deps.extend(st["r"])
        return deps

    def _emit_waits(self, eng, deps):
        need = {}
        for d in deps:
            if d[0] == "e":
                _, g, idx = d
                if g == eng and eng in ("pe", "sp"):
                    continue
                key = ("e", g)
                semh = self.sem[g]
            elif d[0] == "w":
                _, j, idx = d
                key = ("w", j)
                semh = self.swsem[j]
            else:
                _, j, idx = d
                key = ("d", j)
                semh = self.dsem[j]
            if self.seen[eng].get(key, 0) >= idx:
                continue
            if key not in need or need[key][1] < idx:
                need[key] = (semh, idx)
        for key, (semh, idx) in need.items():
            self.seen[eng][key] = idx
            self.ops[eng].append(lambda e, s=semh, v=idx: e.wait_ge(s, v))
            self.ninst += 1

    def _update(self, token, reads, writes):
        for r in reads:
            st = self.res.setdefault(r, {"w": None, "r": []})
            st["r"].append(token)
            if len(st["r"]) > 64:
                st["r"] = st["r"][-48:]
        for w in writes:
            self.res[w] = {"w": token, "r": []}

    def op(self, eng, fn, reads=(), writes=()):
        deps = self._deps(reads, writes)
        self._emit_waits(eng, deps)
        self.cnt[eng] += 1
        idx = self.cnt[eng]
        semh = self.sem[eng]
        self.ops[eng].append(lambda e, f=fn, s=semh: f(e).then_inc(s, 1))
        self.ninst += 1
        self._update(("e", eng, idx), reads, writes)
        return idx

    def dma(self, q, fn, reads=(), writes=()):
        if q == "pool":
            return self._dma_sw(fn, reads, writes)
        deps = self._deps(reads, writes)
        j = self.dnext
        self.dnext = (self.dnext + 1) % len(self.dsem)
        if self.dval[j] > 0:
            deps.append(("d", j, self.dval[j]))
        self._emit_waits(q, deps)
        self.dval[j] += 16
        val = self.dval[j]
        semh = self.dsem[j]
        self.ops[q].append(lambda e, f=fn, s=semh: f(e).then_inc(s, 16))
        self.ninst += 1
        tok = ("d", j, val)
        self._update(tok, reads, writes)
        return tok

    def _dma_sw(self, fn, reads, writes):
        j = self.sw_next
        self.sw_next = (self.sw_next + 1) % len(self.swsem)
        deps = self._deps(reads, writes)
        if self.swval[j] > 0:
            deps.append(("w", j, self.swval[j]))
        self._emit_waits("pool", deps)
        self.swval[j] += 16
        val = self.swval[j]
        semh = self.swsem[j]
        self.ops["pool"].append(lambda e, f=fn, s=semh: f(e).then_inc(s, 16))
        self.ninst += 1
        tok = ("w", j, val)
        self._update(tok, reads, writes)
        return tok

    def barrier(self):
        for j in range(len(self.swsem)):
            self._retire(j)
        deps = [("d", j, v) for j, v in enumerate(self.dval) if v > 0]
        deps += [("w", j, v) for j, v in enumerate(self.swval) if v > 0]
        deps += [("e", g, self.cnt[g]) for g in ENGS if self.cnt[g] > 0]
        for e in ENGS:
            self._emit_waits(e, deps)
        self.res = {}

    def flush(self):
        nc = self.nc
        ops = self.ops
        with nc.Block() as block:
            @block.tensor
            def _(e):
                for f in ops["pe"]:
                    f(e)

            @block.scalar
            def _(e):
                for f in ops["act"]:
                    f(e)

            @block.vector
            def _(e):
                for f in ops["dve"]:
                    f(e)

            @block.gpsimd
            def _(e):
                for f in ops["pool"]:
                    f(e)

            @block.sync
            def _(e):
                for f in ops["sp"]:
                    f(e)
        self.ops = {e: [] for e in ENGS}


class Rot:
    def __init__(self, name, bufs):
        self.name = name
        self.bufs = bufs
        self.i = 0

    def next(self):
        b = self.bufs[self.i % len(self.bufs)]
        r = "%s#%d" % (self.name, self.i % len(self.bufs))
        self.i += 1
        return b, r


def build(debug_stop=None):
    nc = bass.Bass("TRN2", target_bir_lowering=False)

    SLIM = ("x_p", "p_p", "p_s", "w_gate", "w_out", "w_attn_br", "w_ssm_br", "ws1", "ws2", "ws3", "w_ple_gate", "w_ple",
            "w_router", "c_rope", "st_ssm")

    SLIM2 = ("p_p", "p_s", "w_gate", "w_out", "w_attn_br", "w_ssm_br", "ws1", "ws2", "ws3", "w_ple_gate", "w_ple",
             "w_router", "st_ssm", "cache0", "cache1", "cache2", "x_s")
    bis = debug_stop if (debug_stop is not None and 1 < debug_stop < 2) else None

    def din(name, shape, dt=F32):
        if debug_stop is not None and debug_stop <= 0.5 and name in SLIM:
            shape = [1] * len(shape)
        if debug_stop is not None and 1 < debug_stop < 2 and name in SLIM2:
            shape = [1] * len(shape)
        return nc.dram_tensor(name, list(shape), dt, kind="ExternalInput").ap()

    def dout(name, shape, dt=F32):
        return nc.dram_tensor(name, list(shape), dt, kind="ExternalOutput").ap()

    def dscr(name, shape, dt=F32, dbg=False):
        if debug_stop is not None and dbg:
            return nc.dram_tensor(name, list(shape), dt, kind="ExternalOutput").ap()
        return nc.dram_tensor(name, list(shape), dt).ap()

    x_p = din("x_p", [S, D])
    x_s = din("x_s", [NS, D])
    cache = [din("cache%d" % g, [NS, KEEP[g], 512]) for g in range(3)]
    st_ssm = din("st_ssm", [NS, 2048])
    p_p = din("p_p", [S, 256])
    p_s = din("p_s", [NS, 256])
    w_in = din("w_in", [D, 2560])
    a_re = din("a_re", [16, 64]); a_im = din("a_im", [16, 64]); log_dt = din("log_dt", [16])
    b_re = din("b_re", [16, 64, 16]); b_im = din("b_im", [16, 64, 16])
    c_re = din("c_re", [16, 16, 64]); c_im = din("c_im", [16, 16, 64])
    d_skip = din("d_skip", [256]); w_glu = din("w_glu", [256, 256]); b_glu = din("b_glu", [256])
    w_attn_br = din("w_attn_br", [256, D]); w_ssm_br = din("w_ssm_br", [256, D])
    w_gate = din("w_gate", [D, 2 * D]); b_gate = din("b_gate", [2 * D])
    w_out = din("w_out", [D, D])
    ln1_g = din("ln1_g", [D]); ln1_b = din("ln1_b", [D])
    w_router = din("w_router", [D, NEXP]); router_bias = din("router_bias", [NEXP])
    NE_IN = NEXP if debug_stop is None or debug_stop >= 4 else 1
    w1 = din("w1", [NE_IN, D, 256]); w3 = din("w3", [NE_IN, D, 256]); w2 = din("w2", [NE_IN, 256, D])
    ws1 = din("ws1", [D, 256]); ws3 = din("ws3", [D, 256]); ws2 = din("ws2", [256, D])
    w_ple_gate = din("w_ple_gate", [D, D]); w_ple = din("w_ple", [256, D])
    ln2_g = din("ln2_g", [D]); ln2_b = din("ln2_b", [D])
    c_ident = din("c_ident", [128, 128])
    c_rope = din("c_rope", [3, S, 64])
    c_rope_s = din("c_rope_s", [1, 64])
    c_amask = din("c_amask", [128, 1024])
    c_utri = din("c_utri", [128, 128])
    c_iota = din("c_iota", [128, 64])
    c_sel = din("c_sel", [16, 128])
    c_selT = din("c_selT", [128, 16])

    y_p = dout("y_p", [S, D]); y_s = dout("y_s", [NS, D])
    kv_p = [dout("kv_p%d" % g, [KEEP[g], 512]) for g in range(3)]
    kv_s = [dout("kv_s%d" % g, [NS, 512]) for g in range(3)]
    ssm_p = dout("ssm_p", [1024, 2]); ssm_s = dout("ssm_s", [NS, 2048])

    nd_scr = [dscr("nd_scr%d" % g, [S, 260], dbg=True) for g in range(3)]
    x1_scr = dscr("x1_scr", [S + NS, D])
    r_scr = dscr("r_scr", [S + NS, D])
    xg_scr = dscr("xg_scr", [NEXP * CAP, D], BF16)
    yg_scr = dscr("yg_scr", [NEXP * CAP, D], BF16)

    top = ExitStack()
    with top:
        P = Prog(nc, top)

        def sb(stack, name, shape, dt):
            return stack.enter_context(nc.sbuf_tensor(name, list(shape), dt))

        def psum(stack, name, shape, dt):
            return stack.enter_context(nc.psum_tensor(name, list(shape), dt))

        def MM(out, lhsT, rhs, start, stop, reads, writes):
            P.op("pe", lambda e: e.matmul(out, lhsT=lhsT, rhs=rhs, start=start, stop=stop), reads, writes)

        def TR(out, in_, ident, reads, writes):
            P.op("pe", lambda e: e.transpose(out=out, in_=in_, identity=ident), reads, writes)

        def ACT(out, in_, func, reads, writes, bias=None, scale=None, eng="act"):
            kw = {}
            if bias is not None:
                kw["bias"] = bias
            if scale is not None:
                kw["scale"] = scale
            P.op("act", lambda e: e.activation(out=out, in_=in_, func=func, **kw), reads, writes)

        def TT(eng, out, in0, in1, op, reads, writes):
            P.op(eng, lambda e: e.tensor_tensor(out=out, in0=in0, in1=in1, op=op), reads, writes)

        def TS(eng, out, in0, s1, s2, op0, op1, reads, writes):
            if op1 is None:
                P.op(eng, lambda e: e.tensor_scalar(out=out, in0=in0, scalar1=s1, scalar2=None, op0=op0), reads, writes)
            else:
                P.op(eng, lambda e: e.tensor_scalar(out=out, in0=in0, scalar1=s1, scalar2=s2, op0=op0, op1=op1), reads, writes)

        def STT(eng, out, in0, scalar, in1, op0, op1, reads, writes):
            P.op(eng, lambda e: e.scalar_tensor_tensor(out=out, in0=in0, scalar=scalar, in1=in1, op0=op0, op1=op1), reads, writes)

        def CP(eng, out, in_, reads, writes):
            if eng == "act":
                P.op("act", lambda e: e.activation(out=out, in_=in_, func=AF.Copy), reads, writes)
            else:
                P.op(eng, lambda e: e.tensor_copy(out=out, in_=in_), reads, writes)

        def MSET(eng, ap, val, writes):
            P.op(eng, lambda e: e.memset(ap, val), (), writes)

        def DMA(q, out, in_, reads, writes, **kw):
            return P.dma(q, lambda e: e.dma_start(out=out, in_=in_, **kw), reads, writes)

        ident_f = sb(top, "ident_f", [128, 128], F32)
        ident_b = sb(top, "ident_b", [128, 128], BF16)
        vecs = sb(top, "vecs", [128, 20], F32)
        rbias_t = sb(top, "rbias_t", [128, NEXP], F32)
        dest_t = sb(top, "dest_t", [128, NT + 1, 8], I32)
        gate_t = sb(top, "gate_t", [128, NT + 1, 8], F32)
        eps_t = sb(top, "eps_t", [128, 1], F32)

        DMA("sp", ident_f[:], c_ident, (), ["ident_f"])
        CP("dve", ident_b[:], ident_f[:], ["ident_f"], ["ident_b"])
        DMA("sp", rbias_t[:], router_bias.partition_broadcast(128), (), ["rbias"])
        MSET("dve", eps_t[:], LN_EPS, ["eps"])
        MSET("dve", gate_t[:], 0.0, ["gate_t"])
        MSET("dve", dest_t[:], 2000000, ["dest_t"])

        stB = top.enter_context(ExitStack())
        uT = sb(stB, "uT", [128, 2, S], BF16)
        uT_s = sb(stB, "uT_s", [128, 2, NS], BF16)
        bbT = [sb(stB, "bbT%d" % i, [128, 8, 128], BF16) for i in range(2)]
        cT = [sb(stB, "cT%d" % i, [128, 8, 128], BF16) for i in range(2)]
        diagD = sb(stB, "diagD", [128, 2, 128], BF16)
        par = sb(stB, "par", [128, 16, 8], F32)
        carry = sb(stB, "carry", [128, 8, 2], F32)
        attn_oT_s = sb(stB, "attn_oT_s", [128, 2, NS], BF16)

        AR, AI, LDT, DT, ARDT, TH, RHO, COS, SIN, ABR, ABI, CRE, CIM, T0, T1, T2 = range(16)

        def pr(i):
            return par[:, i, :]

        with ExitStack() as st0:
            stage = sb(st0, "stage", [20, 128], F32)
            stage2 = sb(st0, "stage2", [8, 3, 128], F32)
            ldt8 = sb(st0, "ldt8", [8, 2], F32)
            pz = psum(st0, "pz", [128, 4, 128], F32)
            pz2 = psum(st0, "pz2", [128, 512], F32)
            bre = sb(st0, "bre", [128, 8, 16], F32); bim = sb(st0, "bim", [128, 8, 16], F32)
            bbr = sb(st0, "bbr", [128, 8, 16], F32); bbi = sb(st0, "bbi", [128, 8, 16], F32)
            tb1 = sb(st0, "tb1", [128, 8, 16], F32); tb2 = sb(st0, "tb2", [128, 8, 16], F32)
            Yst = [sb(st0, "Yst%d" % i, [128, 8, 128], F32) for i in range(2)]
            Xc = [sb(st0, "Xc%d" % i, [128, 8, 128], F32) for i in range(2)]
            qi_t = sb(st0, "qi_t", [128, 8], I32)

            DMA("sp", stage[0:2, :], d_skip.rearrange("(n p) -> n p", p=128), (), ["stage"])
            DMA("sp", stage[2:4, :], b_glu.rearrange("(n p) -> n p", p=128), (), ["stage"])
            DMA("sp", stage[4:20, :], b_gate.rearrange("(n p) -> n p", p=128), (), ["stage"])
            TR(pz2[:, 0:20], stage[:, :], ident_f[0:20, 0:20], ["stage", "ident_f"], ["pz2"])
            CP("dve", vecs[:], pz2[:, 0:20], ["pz2"], ["vecs"])

            DMA("sp", stage2[:, 0, :], a_re.rearrange("(st gl) p -> st (gl p)", gl=2), (), ["stage2a"])
            DMA("sp", stage2[:, 1, :], a_im.rearrange("(st gl) p -> st (gl p)", gl=2), (), ["stage2b"])
            DMA("sp", ldt8[:], log_dt.rearrange("(st gl) -> st gl", gl=2), (), ["ldt8"])
            CP("dve", stage2[:, 2, :].rearrange("s (gl p) -> s gl p", gl=2), ldt8[:].unsqueeze(2).to_broadcast([8, 2, 64]),
               ["ldt8"], ["stage2c"])
            for i in range(3):
                TR(pz2[:, 32 + 8 * i:40 + 8 * i], stage2[:, i, :], ident_f[0:8, 0:8],
                   ["stage2a", "stage2b", "stage2c", "ident_f"], ["pz2"])
            CP("dve", par[:, 0:3, :], pz2[:, 32:56].rearrange("p (a b) -> p a b", a=3), ["pz2"], ["par"])
            R = ["par"]
            ACT(pr(DT), pr(LDT), AF.Exp, R, R)
            TT("dve", pr(ARDT), pr(AR), pr(DT), ALU.mult, R, R)
            TT("dve", pr(TH), pr(AI), pr(DT), ALU.mult, R, R)
            ACT(pr(RHO), pr(ARDT), AF.Exp, R, R)

            def sin_of(dst, src_row, shift):
                TS("dve", pr(T0), pr(src_row), shift, 1.0 / TWO_PI, ALU.add, ALU.mult, R, R)
                CP("dve", qi_t[:], pr(T0), R, ["qi_t"])
                CP("dve", pr(T1), qi_t[:], ["qi_t"], R)
                TS("dve", pr(T0), pr(src_row), shift, None, ALU.add, None, R, R)
                STT("dve", pr(T0), pr(T1), -TWO_PI, pr(T0), ALU.mult, ALU.add, R, R)
                P.op("dve", lambda e: e.tensor_single_scalar(out=pr(T1), in_=pr(T0), scalar=math.pi, op=ALU.is_gt), R, R)
                STT("dve", pr(T0), pr(T1), -TWO_PI, pr(T0), ALU.mult, ALU.add, R, R)
                P.op("dve", lambda e: e.tensor_single_scalar(out=pr(T1), in_=pr(T0), scalar=-math.pi, op=ALU.is_lt), R, R)
                STT("dve", pr(T0), pr(T1), TWO_PI, pr(T0), ALU.mult, ALU.add, R, R)
                ACT(dst, pr(T0), AF.Sin, R, R)

            sin_of(pr(SIN), TH, 0.0)
            sin_of(pr(COS), TH, math.pi / 2.0)
            TT("dve", pr(ABR), pr(RHO), pr(COS), ALU.mult, R, R)
            TT("dve", pr(ABI), pr(RHO), pr(SIN), ALU.mult, R, R)
            TT("dve", pr(T0), pr(AR), pr(AR), ALU.mult, R, R)
            TT("dve", pr(T1), pr(AI), pr(AI), ALU.mult, R, R)
            TT("dve", pr(T0), pr(T0), pr(T1), ALU.add, R, R)
            P.op("dve", lambda e: e.reciprocal(out=pr(T2), in_=pr(T0)), R, R)
            TS("dve", pr(T0), pr(ABR), -1.0, None, ALU.add, None, R, R)
            TT("dve", pr(CRE), pr(T0), pr(AR), ALU.mult, R, R)
            TT("dve", pr(T1), pr(ABI), pr(AI), ALU.mult, R, R)
            TT("dve", pr(CRE), pr(CRE), pr(T1), ALU.add, R, R)
            TT("dve", pr(CRE), pr(CRE), pr(T2), ALU.mult, R, R)
            TT("dve", pr(CIM), pr(ABI), pr(AR), ALU.mult, R, R)
            TT("dve", pr(T1), pr(T0), pr(AI), ALU.mult, R, R)
            TT("dve", pr(CIM), pr(CIM), pr(T1), ALU.subtract, R, R)
            TT("dve", pr(CIM), pr(CIM), pr(T2), ALU.mult, R, R)

            DMA("sp", bre[:], b_re.rearrange("(st gl) p c -> (gl p) st c", gl=2), (), ["bre"])
            DMA("sp", bim[:], b_im.rearrange("(st gl) p c -> (gl p) st c", gl=2), (), ["bim"])
            cre_b = pr(CRE).unsqueeze(2).to_broadcast([128, 8, 16])
            cim_b = pr(CIM).unsqueeze(2).to_broadcast([128, 8, 16])
            TT("dve", tb1[:], bre[:], cre_b, ALU.mult, ["bre"] + R, ["tb1"])
            TT("dve", tb2[:], bim[:], cim_b, ALU.mult, ["bim"] + R, ["tb2"])
            TT("dve", bbr[:], tb1[:], tb2[:], ALU.subtract, ["tb1", "tb2"], ["bbr"])
            TT("dve", tb1[:], bim[:], cre_b, ALU.mult, ["bim"] + R, ["tb1"])
            TT("dve", tb2[:], bre[:], cim_b, ALU.mult, ["bre"] + R, ["tb2"])
            TT("dve", bbi[:], tb1[:], tb2[:], ALU.add, ["tb1", "tb2"], ["bbi"])
            for i, src in enumerate((bbr, bbi)):
                MSET("pool", Yst[i][:], 0.0, ["Yst%d" % i])
                for st_ in range(8):
                    for gl in range(2):
                        off = 32 * (st_ % 4) + 16 * gl
                        CP("dve", Yst[i][64 * gl:64 * gl + 64, st_, off:off + 16], src[64 * gl:64 * gl + 64, st_, :],
                           ["bbr", "bbi", "Yst%d" % i], ["Yst%d" % i])
                for half in range(2):
                    for j in range(4):
                        st_ = 4 * half + j
                        TR(pz[:, j, :], Yst[i][:, st_, :], ident_f[:], ["Yst%d" % i, "ident_f"], ["pz"])
                    CP("act", bbT[i][:, 4 * half:4 * half + 4, :], pz[:], ["pz"], ["bbT%d" % i])
            for i, src in enumerate((c_re, c_im)):
                MSET("pool", Xc[i][:], 0.0, ["Xc%d" % i])
                for st_ in range(8):
                    for gl in range(2):
                        off = 32 * (st_ % 4) + 16 * gl
                        DMA("sp", Xc[i][off:off + 16, st_, 64 * gl:64 * gl + 64], src[2 * st_ + gl], ["Xc%d" % i], ["Xc%d" % i])
                for half in range(2):
                    for j in range(4):
                        st_ = 4 * half + j
                        TR(pz[:, j, :], Xc[i][:, st_, :], ident_f[:], ["Xc%d" % i, "ident_f"], ["pz"])
                    ACT(cT[i][:, 4 * half:4 * half + 4, :], pz[:], AF.Copy, ["pz"], ["cT%d" % i], scale=(1.0 if i == 0 else -1.0))
            for ck in range(2):
                TS("dve", diagD[:, ck, :], ident_f[:], vecs[:, ck:ck + 1], None, ALU.mult, None, ["ident_f", "vecs"], ["diagD"])
            MSET("dve", carry[:], 0.0, ["carry"])
            if debug_stop == 0:
                dbg_par = dout("dbg_par", [128, 16 * 8])
                dbg_bbT = dout("dbg_bbT", [128, 4, 8 * 128], BF16)
                DMA("sp", dbg_par, par[:].rearrange("p a b -> p (a b)"), ["par"], ["dbg_par"])
                for i_, t_ in enumerate((bbT[0], bbT[1], cT[0], cT[1])):
                    DMA("sp", dbg_bbT[:, i_, :], t_[:].rearrange("p a b -> p (a b)"), ["bbT0", "bbT1", "cT0", "cT1"], ["dbg_bbT"])
            P.barrier()
            P.flush()
            if debug_stop == 0:
                return nc, {}

        with ExitStack() as st1:
            w_in_b = sb(st1, "w_in_b", [128, 8, 2560], BF16)
            w_in_v = w_in.rearrange("(kc p) n -> p kc n", p=128)
            for g in range(3):
                for part in range(3):
                    DMA("pool", w_in_b[:, :, 768 * g + 256 * part:768 * g + 256 * part + 256],
                        w_in_v[:, :, 768 * part + 256 * g:768 * part + 256 * g + 256], (), ["w_in_b"])
            DMA("pool", w_in_b[:, :, 2304:2560], w_in_v[:, :, 2304:2560], (), ["w_in_b"])
            amask = sb(st1, "amask", [128, 1024], BF16)
            nd_s = sb(st1, "nd_s", [NS, 3, 260], F32)

            with ExitStack() as sts:
              if not bis:
                  xs_f = sb(sts, "xs_f", [NS, D], F32)
                  xs_b = sb(sts, "xs_b", [NS, D], BF16)
                  xsT = sb(sts, "xsT", [128, 8, NS], BF16)
                  pj = psum(sts, "pj_s", [NS, 5, 512], F32)
                  pt_full = psum(sts, "pt_s", [128, 1024], BF16)
                  pt = pt_full[:, 0:8 * NS].rearrange("p (a b) -> p a b", a=8)
                  pq_full = psum(sts, "pq_s", [128, 512], F32)
                  pq = pq_full[:, 0:256]
                  pn_full = psum(sts, "pn_s", [128, 512], F32)
                  pn = pn_full[0:NS, 0:260]
                  rope_s = sb(sts, "rope_s", [NS, 64], F32)
                  qk_s = sb(sts, "qk_s", [NS, 3, 8, 64], F32)
                  v_s = sb(sts, "v_s", [NS, 3, 256], F32)
                  u_sb = sb(sts, "u_sb", [NS, 256], BF16)
                  kvo = sb(sts, "kvo", [NS, 3, 512], F32)
                  tq = [sb(sts, "tq%d" % i, [NS, 1, 8, 32], F32) for i in range(4)]
                  sel = sb(sts, "sel", [16, 128], F32)
                  selT = sb(sts, "selT", [128, 16], F32)
                  ctile = sb(sts, "ctile", [128, 16, 512], F32)
                  prod = sb(sts, "prod", [128, 16, 256], F32)
                  qb = sb(sts, "qb", [128, 256], F32)
                  lg = sb(sts, "lg", [128, 16, 4], F32)
                  pex = sb(sts, "pex", [128, 16, 4], F32)
                  part = sb(sts, "part", [128, 4, 65], F32)
                  lgn = sb(sts, "lgn", [NS, 4], F32)
                  pnew = sb(sts, "pnew", [NS, 4], F32)
                  tn = sb(sts, "tn", [NS, 4, 64], F32)
                  rden = sb(sts, "rden", [NS, 4], F32)
                  o_s = sb(sts, "o_s", [NS, 4, 64], F32)
                  o_sb = sb(sts, "o_sb", [NS, 256], BF16)

                  DMA("sp", xs_f[:], x_s, (), ["xs_f"])
                  DMA("sp", rope_s[:], c_rope_s[0].partition_broadcast(NS), (), ["rope_s"])
                  DMA("sp", sel[:], c_sel, (), ["sel"])
                  DMA("sp", selT[:], c_selT, (), ["selT"])
                  selb = sb(sts, "selb", [16, 128], BF16)
                  selTb = sb(sts, "selTb", [128, 16], BF16)
                  q_hi = sb(sts, "q_hi", [NS, 256], BF16); q_lo = sb(sts, "q_lo", [NS, 256], BF16)
                  p_hi = sb(sts, "p_hi", [128, 260], BF16); p_lo = sb(sts, "p_lo", [128, 260], BF16)
                  CP("dve", selb[:], sel[:], ["sel"], ["selb"])
                  CP("dve", selTb[:], selT[:], ["selT"], ["selTb"])
                  if debug_stop == 0.21:
                      P.barrier()
                      P.flush()
                      return nc, {}
                  CP("act", xs_b[:], xs_f[:], ["xs_f"], ["xs_b"])
                  for kc in range(8):
                      TR(pt[:, kc, :], xs_b[:, kc * 128:(kc + 1) * 128], ident_b[0:NS, 0:NS], ["xs_b", "ident_b"], ["pt_s"])
                  CP("dve", xsT[:], pt, ["pt_s"], ["xsT"])
                  if debug_stop == 0.22:
                      P.barrier()
                      P.flush()
                      return nc, {}
                  for ch in range(5):
                      for kc in range(8):
                          MM(pj[:, ch, :], xsT[:, kc, :], w_in_b[:, kc, 512 * ch:512 * ch + 512], kc == 0, kc == 7,
                             ["xsT", "w_in_b"], ["pj_s"])
                  pj_sb = sb(sts, "pj_sb", [NS, 2560], F32)
                  for ch in range(5):
                      CP("act" if ch % 2 else "dve", pj_sb[:, 512 * ch:512 * ch + 512], pj[:, ch, :], ["pj_s"], ["pj_s"])
                  pjf = pj_sb[:]
                  if debug_stop == 0.23:
                      P.barrier()
                      P.flush()
                      return nc, {}
                  for g in range(3):
                      qk = pjf[:, 768 * g:768 * g + 512].rearrange("p (h d) -> p h d", h=8)
                      x1 = qk[:, :, 0:32]; x2 = qk[:, :, 32:64]
                      cs = rope_s[:, 0:32].unsqueeze(1).to_broadcast([NS, 8, 32])
                      sn = rope_s[:, 32:64].unsqueeze(1).to_broadcast([NS, 8, 32])
                      TT("dve", tq[0][:, 0], x1, cs, ALU.mult, ["pj_s", "rope_s"], ["tq0"])
                      TT("dve", tq[1][:, 0], x2, sn, ALU.mult, ["pj_s", "rope_s"], ["tq1"])
                      TT("dve", tq[2][:, 0], x2, cs, ALU.mult, ["pj_s", "rope_s"], ["tq2"])
                      TT("dve", tq[3][:, 0], x1, sn, ALU.mult, ["pj_s", "rope_s"], ["tq3"])
                      TT("dve", qk_s[:, g, :, 0:32], tq[0][:, 0], tq[1][:, 0], ALU.subtract, ["tq0", "tq1"], ["qk_s"])
                      TT("dve", qk_s[:, g, :, 32:64], tq[2][:, 0], tq[3][:, 0], ALU.add, ["tq2", "tq3"], ["qk_s"])
                      CP("act", v_s[:, g, :], pjf[:, 768 * g + 512:768 * g + 768], ["pj_s"], ["v_s"])
                      CP("dve", kvo[:, g, 0:256], qk_s[:, g, 4:8, :].rearrange("p h d -> p (h d)"), ["qk_s"], ["kvo"])
                      CP("dve", kvo[:, g, 256:512], v_s[:, g, :], ["v_s"], ["kvo"])
                      DMA("sp", kv_s[g], kvo[:, g, :], ["kvo"], ["kv_s%d" % g])
                  if debug_stop == 0.24:
                      P.barrier()
                      P.flush()
                      return nc, {}
                  CP("act", u_sb[:], pjf[:, 2304:2560], ["pj_s"], ["u_sb"])
                  for ck in range(2):
                      TR(pt[:, ck, :], u_sb[:, ck * 128:(ck + 1) * 128], ident_b[0:NS, 0:NS], ["u_sb", "ident_b"], ["pt_s"])
                  CP("dve", uT_s[:], pt[:, 0:2, :], ["pt_s"], ["uT_s"])
                  if debug_stop == 0.25:
                      P.barrier()
                      P.flush()
                      return nc, {}

                  for g in range(3):
                      dil = DILS[g]
                      src = cache[g].rearrange("s (c kk dd) f -> (s c) kk dd f", c=8, kk=16, dd=dil)[:, :, 0, :]
                      DMA("sp", ctile[:], src, ["ctile"], ["ctile"])
                      qg = qk_s[:, g, 0:4, :].rearrange("p h d -> p (h d)")
                      CP("dve", q_hi[:], qg, ["qk_s"], ["q_hi"])
                      TT("dve", q_lo[:], qg, q_hi[:], ALU.subtract, ["qk_s", "q_hi"], ["q_lo"])
                      MM(pq, selb[:], q_hi[:], True, False, ["selb", "q_hi"], ["pq_s"])
                      MM(pq, selb[:], q_lo[:], False, True, ["selb", "q_lo"], ["pq_s"])
                      CP("act", qb[:], pq, ["pq_s"], ["qb"])
                      TT("dve", prod[:], ctile[:, :, 0:256], qb[:].unsqueeze(1).to_broadcast([128, 16, 256]), ALU.mult,
                         ["ctile", "qb"], ["prod"])
                      P.op("dve", lambda e: e.tensor_reduce(out=lg[:], in_=prod[:].rearrange("p k (h d) -> p k h d", h=4),
                                                            axis=AX.X, op=ALU.add), ["prod"], ["lg"])
                      ACT(pex[:], lg[:], AF.Exp, ["lg"], ["pex"], scale=0.125)
                      TT("dve", prod[:].rearrange("p k (h d) -> p k h d", h=4),
                         ctile[:, :, 256:512].rearrange("p k (h d) -> p k h d", h=4),
                         pex[:].unsqueeze(3).to_broadcast([128, 16, 4, 64]), ALU.mult, ["ctile", "pex", "prod"], ["prod"])
                      P.op("dve", lambda e: e.tensor_reduce(out=part[:, :, 0:64], in_=prod[:].rearrange("p k (h d) -> p h d k", h=4),
                                                            axis=AX.X, op=ALU.add), ["prod"], ["part"])
                      P.op("dve", lambda e: e.tensor_reduce(out=part[:, :, 64], in_=pex[:].rearrange("p k h -> p h k"),
                                                            axis=AX.X, op=ALU.add), ["pex", "part"], ["part"])
                      partf = part[:].rearrange("p h d -> p (h d)")
                      CP("dve", p_hi[:], partf, ["part"], ["p_hi"])
                      TT("dve", p_lo[:], partf, p_hi[:], ALU.subtract, ["part", "p_hi"], ["p_lo"])
                      MM(pn, selTb[:], p_hi[:], True, False, ["selTb", "p_hi"], ["pn_s"])
                      MM(pn, selTb[:], p_lo[:], False, True, ["selTb", "p_lo"], ["pn_s"])
                      TT("dve", tn[:], qk_s[:, g, 0:4, :], qk_s[:, g, 4:8, :], ALU.mult, ["qk_s"], ["tn"])
                      P.op("dve", lambda e: e.tensor_reduce(out=lgn[:], in_=tn[:], axis=AX.X, op=ALU.add), ["tn"], ["lgn"])
                      ACT(pnew[:], lgn[:], AF.Exp, ["lgn"], ["pnew"], scale=0.125)
                      TT("dve", tn[:], v_s[:, g, :].rearrange("p (h d) -> p h d", h=4),
                         pnew[:].unsqueeze(2).to_broadcast([NS, 4, 64]), ALU.mult, ["v_s", "pnew", "tn"], ["tn"])
                      pnv = pn.rearrange("p (h d) -> p h d", h=4)
                      ndv = nd_s[:, g, :].rearrange("p (h d) -> p h d", h=4)
                      TT("dve", ndv[:, :, 0:64], pnv[:, :, 0:64], tn[:], ALU.add, ["pn_s", "tn"], ["nd_s"])
                      TT("dve", ndv[:, :, 64:65], pnv[:, :, 64:65], pnew[:].unsqueeze(2), ALU.add, ["pn_s", "pnew", "nd_s"], ["nd_s"])
                  nds = nd_s[:, 0, :]
                  TT("dve", nds, nd_s[:, 0, :], nd_s[:, 1, :], ALU.add, ["nd_s"], ["nd_s"])
                  TT("dve", nds, nd_s[:, 0, :], nd_s[:, 2, :], ALU.add, ["nd_s"], ["nd_s"])
                  ndv = nd_s[:, 0, :].rearrange("p (h d) -> p h d", h=4)
                  P.op("dve", lambda e: e.reciprocal(out=rden[:], in_=ndv[:, :, 64:65].rearrange("p h o -> p (h o)")), ["nd_s"], ["rden"])
                  TT("dve", o_s[:], ndv[:, :, 0:64], rden[:].unsqueeze(2).to_broadcast([NS, 4, 64]), ALU.mult, ["nd_s", "rden"], ["o_s"])
                  CP("act", o_sb[:], o_s[:].rearrange("p h d -> p (h d)"), ["o_s"], ["o_sb"])
                  for pr_ in range(2):
                      TR(pt[:, pr_, :], o_sb[:, pr_ * 128:(pr_ + 1) * 128], ident_b[0:NS, 0:NS], ["o_sb", "ident_b"], ["pt_s"])
                  CP("dve", attn_oT_s[:], pt[:, 0:2, :], ["pt_s"], ["attn_oT_s"])
                  if debug_stop == 0.5:
                      dbg_nds = dout("dbg_nds", [NS, 3 * 260])
                      DMA("sp", dbg_nds, nd_s[:].rearrange("p a b -> p (a b)"), ["nd_s"], ["dbg_nds"])
                  P.barrier()
                  P.flush()
                  if debug_stop == 0.5:
                      return nc, {}

            with ExitStack() as stp:
                kT_g = sb(stp, "kT_g", [128, 2, S], BF16)
                V_g = sb(stp, "V_g", [128, NT, 4, 65], BF16)
                NB = 2
                x_f = Rot("x_f", [sb(stp, "x_f%d" % i, [128, D], F32) for i in range(NB)])
                x_b = Rot("x_b", [sb(stp, "x_b%d" % i, [128, D], BF16) for i in range(NB)])
                xTt = Rot("xTt", [sb(stp, "xTt%d" % i, [128, 8, 128], BF16) for i in range(NB)])
                ropet = Rot("ropet", [sb(stp, "ropet%d" % i, [128, 64], F32) for i in range(NB)])
                tr_ = [Rot("tr%d" % j, [sb(stp, "tr%d_%d" % (j, i), [128, 8, 32], F32) for i in range(NB)]) for j in range(4)]
                qkr = Rot("qkr", [sb(stp, "qkr%d" % i, [128, 8, 64], F32) for i in range(NB)])
                qkb = Rot("qkb", [sb(stp, "qkb%d" % i, [128, 512], BF16) for i in range(NB)])
                qTt = Rot("qTt", [sb(stp, "qTt%d" % i, [128, 2, 128], BF16) for i in range(NB)])
                vf = Rot("vf", [sb(stp, "vf%d" % i, [128, 256], F32) for i in range(NB)])
                kvt = Rot("kvt", [sb(stp, "kvt%d" % i, [128, 512], F32) for i in range(NB)])
                ub = Rot("ub", [sb(stp, "ub%d" % i, [128, 256], BF16) for i in range(NB)])
                pe_sb = Rot("pe_sb", [sb(stp, "pe_sb%d" % i, [128, 1024], BF16) for i in range(NB)])
                pm_sb = Rot("pm_sb", [sb(stp, "pm_sb%d" % i, [128, 1024], BF16) for i in range(NB)])
                ndt = Rot("ndt", [sb(stp, "ndt%d" % i, [128, 260], F32) for i in range(NB)])
                p_xT = psum(stp, "p_xT", [128, 8, 128], BF16)
                p_qk = psum(stp, "p_qk", [128, 512], F32)
                p_vu = psum(stp, "p_vu", [128, 512], F32)
                p_t = psum(stp, "p_t", [128, 8, 128], BF16)
                p_S = psum(stp, "p_S", [128, 1024], F32)
                p_O_full = psum(stp, "p_O", [128, 512], F32)
                p_O = p_O_full[:, 0:260].rearrange("p (h d) -> p h d", h=4)
                MSET("pool", V_g[:, :, :, 64:65], 1.0, ["V_ones"])
                amask_f = sb(stp, "amask_f", [128, 1024], F32)
                DMA("sp", amask_f[:], c_amask, (), ["amask_f"])
                CP("dve", amask[:], amask_f[:], ["amask_f"], ["amask"])

                for g in range(3):
                    dil = DILS[g]
                    tpc = NT // dil
                    for n in range(NT):
                        r, c = divmod(n, tpc)
                        if bis and (g > 0 or n not in (0, 1, 31)):
                            continue
                        rows = slice(r + 128 * c * dil, r + 128 * c * dil + 127 * dil + 1, dil)
                        xf, xf_r = x_f.next()
                        DMA("sp", xf[:], x_p[rows, :], [xf_r], [xf_r])
                        rt, rt_r = ropet.next()
                        DMA("sp", rt[:], c_rope[g, 128 * n:128 * n + 128, :], [rt_r], [rt_r])
                        xb, xb_r = x_b.next()
                        CP("act", xb[:], xf[:], [xf_r], [xb_r])
                        for kc in range(8):
                            TR(p_xT[:, kc, :], xb[:, kc * 128:(kc + 1) * 128], ident_b[:], [xb_r, "ident_b"], ["p_xT"])
                        xt, xt_r = xTt.next()
                        CP("dve", xt[:], p_xT[:], ["p_xT"], [xt_r])
                        for kc in range(8):
                            MM(p_qk[:], xt[:, kc, :], w_in_b[:, kc, 768 * g:768 * g + 512], kc == 0, kc == 7,
                               [xt_r, "w_in_b"], ["p_qk"])
                        nv = 512 if g == 0 else 256
                        for kc in range(8):
                            if g == 0:
                                MM(p_vu[:, 0:256], xt[:, kc, :], w_in_b[:, kc, 512:768], kc == 0, kc == 7,
                                   [xt_r, "w_in_b"], ["p_vu"])
                            else:
                                MM(p_vu[:, 0:256], xt[:, kc, :], w_in_b[:, kc, 768 * g + 512:768 * g + 768], kc == 0, kc == 7,
                                   [xt_r, "w_in_b"], ["p_vu"])
                        if g == 0:
                            for kc in range(8):
                                MM(p_vu[:, 256:512], xt[:, kc, :], w_in_b[:, kc, 2304:2560], kc == 0, kc == 7,
                                   [xt_r, "w_in_b"], ["p_vu"])
                        if bis and bis <= 1.1:
                            continue
                        qk = p_qk[:].rearrange("p (h d) -> p h d", h=8)
                        x1 = qk[:, :, 0:32]; x2 = qk[:, :, 32:64]
                        cs = rt[:, 0:32].unsqueeze(1).to_broadcast([128, 8, 32])
                        sn = rt[:, 32:64].unsqueeze(1).to_broadcast([128, 8, 32])
                        t = [tr_[j].next() for j in range(4)]
                        TT("dve", t[0][0][:], x1, cs, ALU.mult, ["p_qk", rt_r], [t[0][1]])
                        TT("dve", t[1][0][:], x2, sn, ALU.mult, ["p_qk", rt_r], [t[1][1]])
                        TT("dve", t[2][0][:], x2, cs, ALU.mult, ["p_qk", rt_r], [t[2][1]])
                        TT("dve", t[3][0][:], x1, sn, ALU.mult, ["p_qk", rt_r], [t[3][1]])
                        qr, qr_r = qkr.next()
                        TT("pool", qr[:, :, 0:32], t[0][0][:], t[1][0][:], ALU.subtract, [t[0][1], t[1][1]], [qr_r])
                        TT("pool", qr[:, :, 32:64], t[2][0][:], t[3][0][:], ALU.add, [t[2][1], t[3][1], qr_r], [qr_r])
                        qb_, qb_r = qkb.next()
                        CP("act", qb_[:], qr[:].rearrange("p h d -> p (h d)"), [qr_r], [qb_r])
                        CP("act", V_g[:, n, :, 0:64], p_vu[:, 0:256].rearrange("p (h d) -> p h d", h=4), ["p_vu"], ["V_g%d" % n])
                        if bis and bis <= 1.2:
                            continue
                        is_out = (c == tpc - 1)
                        if is_out:
                            kv, kv_r = kvt.next()
                            CP("act", kv[:, 256:512], p_vu[:, 0:256], ["p_vu"], [kv_r])
                            CP("pool", kv[:, 0:256], qr[:, 4:8, :].rearrange("p h d -> p (h d)"), [qr_r, kv_r], [kv_r])
                            orow = slice(r, r + 127 * dil + 1, dil)
                            DMA("sp", kv_p[g][orow, :], kv[:], [kv_r], ["kv_p%d" % g])
                        if g == 0:
                            u_, u_r = ub.next()
                            CP("act", u_[:], p_vu[:, 256:512], ["p_vu"], [u_r])
                        if bis and bis <= 1.3:
                            continue
                        for j in range(4):
                            TR(p_t[:, j, :], qb_[:, j * 128:(j + 1) * 128], ident_b[:], [qb_r, "ident_b"], ["p_t"])
                        if g == 0:
                            for j in range(2):
                                TR(p_t[:, 4 + j, :], u_[:, j * 128:(j + 1) * 128], ident_b[:], [u_r, "ident_b"], ["p_t"])
                        if bis and bis <= 1.32:
                            continue
                        qt, qt_r = qTt.next()
                        CP("dve", qt[:], p_t[:, 0:2, :], ["p_t"], [qt_r])
                        if bis and bis <= 1.34:
                            continue
                        CP("dve", kT_g[:, :, 128 * n:128 * n + 128], p_t[:, 2:4, :], ["p_t"], ["kT_g%d" % n])
                        if bis and bis <= 1.36:
                            continue
                        if g == 0:
                            CP("dve", uT[:, :, 128 * n:128 * n + 128], p_t[:, 4:6, :], ["p_t"], ["uT%d" % n])
                        if bis and bis <= 1.4:
                            continue
                        kts = ([n - 1] if c > 0 else []) + [n]
                        pSv = p_S[:].rearrange("p (h kt q) -> p h kt q", h=4, kt=2)
                        for h in range(4):
                            for kt_n in kts:
                                kti = 1 if kt_n == n else 0
                                po_ = 64 * (h % 2)
                                MM(pSv[:, 2 * (h % 2) + h // 2, kti, :], kT_g[po_:po_ + 64, h // 2, 128 * kt_n:128 * kt_n + 128],
                                   qt[po_:po_ + 64, h // 2, :], True, True, ["kT_g%d" % kt_n, qt_r], ["p_S"])
                        pe_, pe_r = pe_sb.next()
                        pm_, pm_r = pm_sb.next()
                        pev = pe_[:].rearrange("p (h kt q) -> p h kt q", h=4, kt=2)
                        pmv = pm_[:].rearrange("p (h kt q) -> p h kt q", h=4, kt=2)
                        amv = amask[:].rearrange("p (h kt q) -> p h kt q", h=4, kt=2)
                        if c > 0:
                            ACT(pe_[:, 0:512], p_S[:, 0:512], AF.Exp, ["p_S"], [pe_r], scale=0.125)
                            ACT(pe_[:, 512:1024], p_S[:, 512:1024], AF.Exp, ["p_S", pe_r], [pe_r], scale=0.125)
                            TT("dve", pm_[:], pe_[:], amask[:], ALU.mult, [pe_r, "amask"], [pm_r])
                        else:
                            ACT(pev[:, 0:2, 1, :], pSv[:, 0:2, 1, :], AF.Exp, ["p_S"], [pe_r], scale=0.125)
                            ACT(pev[:, 2:4, 1, :], pSv[:, 2:4, 1, :], AF.Exp, ["p_S", pe_r], [pe_r], scale=0.125)
                            TT("dve", pmv[:, :, 1, :], pev[:, :, 1, :], amv[:, :, 1, :], ALU.mult, [pe_r, "amask"], [pm_r])
                        if bis and bis <= 1.5:
                            continue
                        for h in range(4):
                            for i_k, kt_n in enumerate(kts):
                                kti = 1 if kt_n == n else 0
                                MM(p_O[:, h, :], pmv[:, 2 * (h % 2) + h // 2, kti, :], V_g[:, kt_n, h, :], i_k == 0, i_k == len(kts) - 1,
                                   [pm_r, "V_g%d" % kt_n, "V_ones"], ["p_O"])
                        nd_, nd_r = ndt.next()
                        CP("dve", nd_[:], p_O_full[:, 0:260], ["p_O"], [nd_r])
                        DMA("sp", nd_scr[g][rows, :], nd_[:], [nd_r], ["nd_scr"])
                P.barrier()
                P.flush()
        if debug_stop == 1 or bis:
            return nc, dict(nd_scr=nd_scr)

        NTOK = S + NS
        s2_scr = dscr("s2_scr", [2, 128, NTOK], BF16)
        o_scr = dscr("o_scr", [2, 128, NTOK], BF16)
        NABI = 13
        with ExitStack() as s2a:
            tabC = sb(s2a, "tabC", [128, 8, 256], F32)
            tabS = sb(s2a, "tabS", [128, 8, 256], F32)
            w_glu_b = sb(s2a, "w_glu_b", [128, 2, 256], BF16)
            DMA("pool", w_glu_b[:], w_glu.rearrange("(ck p) n -> p ck n", p=128), (), ["w_glu_b"])
            R = ["par"]
            TS("dve", pr(NABI), pr(ABI), -1.0, None, ALU.mult, None, R, R)
            with ExitStack() as stt:
                tt = [sb(stt, "tt%d" % i, [128, 8, 128], F32) for i in range(4)]
                CP("dve", tabC[:, :, 0:1], pr(COS).unsqueeze(2), R, ["tab"])
                CP("dve", tabS[:, :, 0:1], pr(SIN).unsqueeze(2), R, ["tab"])
                m = 1
                while m < 256:
                    cm = tabC[:, :, m - 1:m].to_broadcast([128, 8, m])
                    sm = tabS[:, :, m - 1:m].to_broadcast([128, 8, m])
                    c0 = tabC[:, :, 0:m]; s0 = tabS[:, :, 0:m]
                    t = [tt[i][:, :, 0:m] for i in range(4)]
                    TT("dve", t[0], c0, cm, ALU.mult, ["tab"], ["tt0"])
                    TT("dve", t[1], s0, sm, ALU.mult, ["tab"], ["tt1"])
                    TT("dve", t[2], s0, cm, ALU.mult, ["tab"], ["tt2"])
                    TT("dve", t[3], c0, sm, ALU.mult, ["tab"], ["tt3"])
                    TT("dve", tabC[:, :, m:2 * m], t[0], t[1], ALU.subtract, ["tt0", "tt1", "tab"], ["tab"])
                    TT("dve", tabS[:, :, m:2 * m], t[2], t[3], ALU.add, ["tt2", "tt3", "tab"], ["tab"])
                    m *= 2
                P.barrier()
                P.flush()
            p_bu = psum(s2a, "p_bu", [128, 2, 512], F32)
            p_y = psum(s2a, "p_y", [128, 2, 512], F32)
            p_z = psum(s2a, "p_z", [128, 2, 512], F32)
            p_tr = psum(s2a, "p_tr", [128, 2, 512], BF16)
            p_h0 = psum(s2a, "p_h0", [128, 512], F32)
            tm = [sb(s2a, "tm%d" % i, [128, 256], F32) for i in range(4)]
            bt = [sb(s2a, "bt%d" % i, [128, 256], F32) for i in range(2)]
            ht = [sb(s2a, "ht%d" % i, [128, 256], F32) for i in range(2)]
            Hb = [sb(s2a, "Hb%d" % i, [128, 512], BF16) for i in range(2)]
            s_f = sb(s2a, "s_f", [128, 2, 512], F32)
            s_b = sb(s2a, "s_b", [128, 2, 512], BF16)
            g1 = sb(s2a, "g1", [128, 2, 512], F32)
            g2 = sb(s2a, "g2", [128, 2, 512], F32)
            s2T = sb(s2a, "s2T", [128, 2, 512], BF16)
            ndl = Rot("ndl", [sb(s2a, "ndl%d" % i, [128, 3, 260], F32) for i in range(2)])
            rdn = sb(s2a, "rdn", [128, 4], F32)
            o_b = Rot("o_b", [sb(s2a, "o_b%d" % i, [128, 256], BF16) for i in range(2)])
            oT_g = sb(s2a, "oT_g", [128, 2, 512], BF16)
            h0s = sb(s2a, "h0s", [NS, 1024, 2], F32)
            h0T = sb(s2a, "h0T", [128, 8, 2, NS], F32)
            hsn = sb(s2a, "hsn", [128, 2, NS], F32)
            hs_out = sb(s2a, "hs_out", [NS, 1024, 2], F32)

            def gelu_glu(ntok, col0):
                for ck in range(2):
                    y = p_y[:, ck, 0:ntok]
                    sf = s_f[:, ck, 0:ntok]; a1 = g1[:, ck, 0:ntok]; a2 = g2[:, ck, 0:ntok]
                    ACT(a1, y, AF.Square, ["p_y"], ["g1"])
                    TS("dve", a1, a1, 0.044715, 1.0, ALU.mult, ALU.add, ["g1"], ["g1"])
                    TT("dve", a1, a1, y, ALU.mult, ["g1", "p_y"], ["g1"])
                    ACT(a2, a1, AF.Sigmoid, ["g1"], ["g2"], scale=1.5957691216057308)
                    TT("dve", sf, a2, y, ALU.mult, ["g2", "p_y"], ["s_f"])
                    CP("act", s_b[:, ck, 0:ntok], sf, ["s_f"], ["s_b"])
                for co in range(2):
                    for ck in range(2):
                        MM(p_z[:, co, 0:ntok], w_glu_b[:, ck, 128 * co:128 * co + 128], s_b[:, ck, 0:ntok], ck == 0, ck == 1,
                           ["w_glu_b", "s_b"], ["p_z"])
                    ACT(g2[:, co, 0:ntok], p_z[:, co, 0:ntok], AF.Sigmoid, ["p_z", "g2"], ["g2"], bias=vecs[:, 2 + co:3 + co])
                TT("pool", s2T[:, :, 0:ntok], s_f[:, :, 0:ntok], g2[:, :, 0:ntok], ALU.mult, ["s_f", "g2"], ["s2T"])
                for ck in range(2):
                    DMA("sp", s2_scr[ck, :, col0:col0 + ntok], s2T[:, ck, 0:ntok], ["s2T"], ["s2_scr"])

            for G in range(8):
                cols = slice(512 * G, 512 * G + 512)
                for ck in range(2):
                    MM(p_y[:, ck, :], diagD[:, ck, :], uT[:, ck, cols], True, False, ["diagD", "uT_all"], ["p_y"])
                    for st_ in range(4 * ck, 4 * ck + 4):
                        MM(p_bu[:, 0, :], bbT[0][:, st_, :], uT[:, ck, cols], True, True, ["bbT0", "uT_all"], ["p_bu"])
                        MM(p_bu[:, 1, :], bbT[1][:, st_, :], uT[:, ck, cols], True, True, ["bbT1", "uT_all"], ["p_bu"])
                        rho_b = par[:, RHO, st_:st_ + 1].to_broadcast([128, 256])
                        cr = "carry%d" % st_
                        for hf in range(2):
                            c0 = 256 * hf
                            b_r = p_bu[:, 0, c0:c0 + 256]; b_i = p_bu[:, 1, c0:c0 + 256]
                            Cc = tabC[:, st_, :]; Sn = tabS[:, st_, :]
                            TT("dve", tm[0][:], b_r, Cc, ALU.mult, ["p_bu", "tab"], ["tm0"])
                            TT("dve", tm[1][:], b_i, Sn, ALU.mult, ["p_bu", "tab"], ["tm1"])
                            TT("dve", tm[2][:], b_i, Cc, ALU.mult, ["p_bu", "tab"], ["tm2"])
                            TT("dve", tm[3][:], b_r, Sn, ALU.mult, ["p_bu", "tab"], ["tm3"])
                            TT("pool", bt[0][:], tm[0][:], tm[1][:], ALU.add, ["tm0", "tm1"], ["bt0"])
                            TT("pool", bt[1][:], tm[2][:], tm[3][:], ALU.subtract, ["tm2", "tm3"], ["bt1"])
                            P.op("dve", lambda e, st_=st_, rho_b=rho_b: e.tensor_tensor_scan(
                                out=ht[0][:], data0=rho_b, data1=bt[0][:], initial=carry[:, st_, 0:1], op0=ALU.mult, op1=ALU.add),
                                ["bt0", cr, "par"], ["ht0"])
                            P.op("dve", lambda e, st_=st_, rho_b=rho_b: e.tensor_tensor_scan(
                                out=ht[1][:], data0=rho_b, data1=bt[1][:], initial=carry[:, st_, 1:2], op0=ALU.mult, op1=ALU.add),
                                ["bt1", cr, "par"], ["ht1"])
                            TT("dve", tm[0][:], ht[0][:], Cc, ALU.mult, ["ht0", "tab"], ["tm0"])
                            TT("pool", tm[1][:], ht[1][:], Sn, ALU.mult, ["ht1", "tab"], ["tm1"])
                            TT("dve", tm[2][:], ht[1][:], Cc, ALU.mult, ["ht1", "tab"], ["tm2"])
                            TT("pool", tm[3][:], ht[0][:], Sn, ALU.mult, ["ht0", "tab"], ["tm3"])
                            TT("pool", Hb[0][:, c0:c0 + 256], tm[0][:], tm[1][:], ALU.subtract, ["tm0", "tm1"], ["Hb0"])
                            TT("pool", Hb[1][:, c0:c0 + 256], tm[2][:], tm[3][:], ALU.add, ["tm2", "tm3"], ["Hb1"])
                            TT("pool", carry[:, st_, 0:1], tm[0][:, 255:256], tm[1][:, 255:256], ALU.subtract, ["tm0", "tm1", cr], [cr])
                            TT("pool", carry[:, st_, 1:2], tm[2][:, 255:256], tm[3][:, 255:256], ALU.add, ["tm2", "tm3", cr], [cr])
                        last = (st_ == 4 * ck + 3)
                        MM(p_y[:, ck, :], cT[0][:, st_, :], Hb[0][:], False, False, ["cT0", "Hb0"], ["p_y"])
                        MM(p_y[:, ck, :], cT[1][:, st_, :], Hb[1][:], False, last, ["cT1", "Hb1"], ["p_y"])
                gelu_glu(512, 512 * G)
                for ti in range(4):
                    n = 4 * G + ti
                    nl, nl_r = ndl.next()
                    for g in range(3):
                        DMA("sp", nl[:, g, :], nd_scr[g][128 * n:128 * n + 128, :], [nl_r], [nl_r])
                    TT("pool", nl[:, 0, :], nl[:, 0, :], nl[:, 1, :], ALU.add, [nl_r], [nl_r])
                    TT("pool", nl[:, 0, :], nl[:, 0, :], nl[:, 2, :], ALU.add, [nl_r], [nl_r])
                    nv = nl[:, 0, :].rearrange("p (h d) -> p h d", h=4)
                    P.op("dve", lambda e, nv=nv: e.reciprocal(out=rdn[:], in_=nv[:, :, 64]), [nl_r], ["rdn"])
                    ob, ob_r = o_b.next()
                    TT("dve", ob[:].rearrange("p (h d) -> p h d", h=4), nv[:, :, 0:64], rdn[:].unsqueeze(2).to_broadcast([128, 4, 64]),
                       ALU.mult, [nl_r, "rdn"], [ob_r])
                    for j in range(2):
                        TR(p_tr[:, j, 128 * ti:128 * ti + 128], ob[:, 128 * j:128 * j + 128], ident_b[:], [ob_r, "ident_b"], ["p_tr"])
                CP("dve", oT_g[:], p_tr[:], ["p_tr"], ["oT_g"])
                for j in range(2):
                    DMA("sp", o_scr[j, :, 512 * G:512 * G + 512], oT_g[:, j, :], ["oT_g"], ["o_scr"])
            DMA("sp", ssm_p.rearrange("(st q) two -> q st two", q=128), carry[:], ["carry%d" % i for i in range(8)], ["ssm_p"])

            DMA("sp", h0s[:], st_ssm.rearrange("s (n two) -> s n two", two=2), (), ["h0s"])
            for st_ in range(8):
                for ri in range(2):
                    TR(p_h0[:, (2 * st_ + ri) * NS:(2 * st_ + ri + 1) * NS], h0s[:, 128 * st_:128 * st_ + 128, ri], ident_f[0:NS, 0:NS],
                       ["h0s", "ident_f"], ["p_h0"])
            CP("dve", h0T[:].rearrange("p a b c -> p (a b c)"), p_h0[:, 0:16 * NS], ["p_h0"], ["h0T"])
            for ck in range(2):
                MM(p_y[:, ck, 0:NS], diagD[:, ck, :], uT_s[:, ck, :], True, False, ["diagD", "uT_s"], ["p_y"])
                for st_ in range(4 * ck, 4 * ck + 4):
                    MM(p_bu[:, 0, 0:NS], bbT[0][:, st_, :], uT_s[:, ck, :], True, True, ["bbT0", "uT_s"], ["p_bu"])
                    MM(p_bu[:, 1, 0:NS], bbT[1][:, st_, :], uT_s[:, ck, :], True, True, ["bbT1", "uT_s"], ["p_bu"])
                    abr = par[:, ABR, st_:st_ + 1]; abi = par[:, ABI, st_:st_ + 1]; nabi = par[:, NABI, st_:st_ + 1]
                    h0r = h0T[:, st_, 0, :]; h0i = h0T[:, st_, 1, :]
                    TS("dve", hsn[:, 0, :], h0r, abr, None, ALU.mult, None, ["h0T", "par"], ["hsn"])
                    STT("dve", hsn[:, 0, :], h0i, nabi, hsn[:, 0, :], ALU.mult, ALU.add, ["h0T", "par", "hsn"], ["hsn"])
                    TT("dve", hsn[:, 0, :], hsn[:, 0, :], p_bu[:, 0, 0:NS], ALU.add, ["hsn", "p_bu"], ["hsn"])
                    TS("dve", hsn[:, 1, :], h0i, abr, None, ALU.mult, None, ["h0T", "par", "hsn"], ["hsn"])
                    STT("dve", hsn[:, 1, :], h0r, abi, hsn[:, 1, :], ALU.mult, ALU.add, ["h0T", "par", "hsn"], ["hsn"])
                    TT("dve", hsn[:, 1, :], hsn[:, 1, :], p_bu[:, 1, 0:NS], ALU.add, ["hsn", "p_bu"], ["hsn"])
                    CP("act", Hb[0][:, 0:NS], hsn[:, 0, :], ["hsn"], ["Hb0"])
                    CP("act", Hb[1][:, 0:NS], hsn[:, 1, :], ["hsn"], ["Hb1"])
                    last = (st_ == 4 * ck + 3)
                    MM(p_y[:, ck, 0:NS], cT[0][:, st_, :], Hb[0][:, 0:NS], False, False, ["cT0", "Hb0"], ["p_y"])
                    MM(p_y[:, ck, 0:NS], cT[1][:, st_, :], Hb[1][:, 0:NS], False, last, ["cT1", "Hb1"], ["p_y"])
                    for ri in range(2):
                        TR(p_h0[0:NS, 256:384], hsn[:, ri, :], ident_f[:], ["hsn", "ident_f"], ["p_h0"])
                        CP("dve", hs_out[:, 128 * st_:128 * st_ + 128, ri], p_h0[0:NS, 256:384], ["p_h0"], ["hs_out"])
            DMA("sp", ssm_s, hs_out[:].rearrange("s n two -> s (n two)"), ["hs_out"], ["ssm_s"])
            gelu_glu(NS, S)
            for j in range(2):
                DMA("sp", o_scr[j, :, S:S + NS], attn_oT_s[:, j, :], ["attn_oT_s"], ["o_scr"])
            if debug_stop == 2:
                dbg_s2 = dout("dbg_s2", [2, 128, NTOK], BF16)
                dbg_o = dout("dbg_o", [2, 128, NTOK], BF16)
                DMA("sp", dbg_s2, s2_scr, ["s2_scr"], ["dbg_s2"])
                DMA("sp", dbg_o, o_scr, ["o_scr"], ["dbg_o"])
            P.barrier()
            P.flush()
        if debug_stop == 2:
            return nc, {}
        stB.close()

        def layer_norm_rows(rows, h, g_t, b_t, stats, mv, rstd, hres):
            for half in range(2):
                P.op("dve", lambda e, half=half: e.bn_stats(out=stats[:rows, half, :], in_=h[:rows, 512 * half:512 * half + 512]), [hres], ["ln_stats"])
            P.op("dve", lambda e: e.bn_aggr(out=mv[:rows, :], in_=stats[:rows, :, :].rearrange("p a b -> p (a b)")), ["ln_stats"], ["ln_mv"])
            ACT(rstd[:rows, :], mv[:rows, 1:2], AF.Sqrt, ["ln_mv", "eps"], ["ln_rstd"], bias=eps_t[:rows, :])
            P.op("dve", lambda e: e.reciprocal(out=rstd[:rows, :], in_=rstd[:rows, :]), ["ln_rstd"], ["ln_rstd"])
            TS("dve", h[:rows, :], h[:rows, :], mv[:rows, 0:1], rstd[:rows, 0:1], ALU.subtract, ALU.mult, [hres, "ln_mv", "ln_rstd"], [hres])
            TT("pool", h[:rows, :], h[:rows, :], g_t[:rows, :], ALU.mult, [hres, "ln_g"], [hres])
            TT("pool", h[:rows, :], h[:rows, :], b_t[:rows, :], ALU.add, [hres, "ln_b"], [hres])

        with ExitStack() as s2b:
            ln1g_t = sb(s2b, "ln1g_t", [128, D], F32); ln1b_t = sb(s2b, "ln1b_t", [128, D], F32)
            DMA("sp", ln1g_t[:], ln1_g.partition_broadcast(128), (), ["ln_g"])
            DMA("sp", ln1b_t[:], ln1_b.partition_broadcast(128), (), ["ln_b"])
            w_gate_b = sb(s2b, "w_gate_b", [128, 8, 2048], BF16)
            w_out_b = sb(s2b, "w_out_b", [128, 8, 1024], BF16)
            w_attn_b = sb(s2b, "w_attn_b", [128, 2, 1024], BF16)
            w_ssmbr_b = sb(s2b, "w_ssmbr_b", [128, 2, 1024], BF16)
            wgv = w_gate.rearrange("(kc p) n -> p kc n", p=128)
            for j in range(4):
                DMA("pool", w_gate_b[:, :, 512 * j:512 * j + 512], wgv[:, :, 512 * j:512 * j + 512], (), ["w_gate_b"])
            wov = w_out.rearrange("(kc p) n -> p kc n", p=128)
            for j in range(2):
                DMA("pool", w_out_b[:, :, 512 * j:512 * j + 512], wov[:, :, 512 * j:512 * j + 512], (), ["w_out_b"])
            DMA("pool", w_attn_b[:], w_attn_br.rearrange("(kc p) n -> p kc n", p=128), (), ["w_attn_b"])
            DMA("pool", w_ssmbr_b[:], w_ssm_br.rearrange("(kc p) n -> p kc n", p=128), (), ["w_ssmbr_b"])
            zt = sb(s2b, "zt", [128, 2048], BF16)
            MSET("pool", zt[:], 0.0, ["zt"])
            xgz = xg_scr.rearrange("(p a two) d -> p a (two d)", p=128, two=2)
            for j in range(8):
                DMA("sp", xgz[:, 32 * j:32 * j + 32, :], zt[:].unsqueeze(1).to_broadcast([128, 32, 2048]), ["zt"], ["xg_zero"])
            p_xT = psum(s2b, "p_xT2", [128, 8, 128], BF16)
            pA = psum(s2b, "pA", [128, 512], F32); pS_ = psum(s2b, "pS_", [128, 512], F32)
            pGA = psum(s2b, "pGA", [128, 512], F32); pGS = psum(s2b, "pGS", [128, 512], F32)
            pM = psum(s2b, "pM", [128, 2, 512], F32)
            x_f4 = sb(s2b, "x_f4", [128, 4, D], F32)
            x_b2 = Rot("x_b2", [sb(s2b, "x_b2_%d" % i, [128, D], BF16) for i in range(2)])
            xT_g = sb(s2b, "xT_g", [128, 8, 512], BF16)
            s2l = sb(s2b, "s2l", [128, 2, 512], BF16); oTl = sb(s2b, "oTl", [128, 2, 512], BF16)
            gA = sb(s2b, "gA", [128, 512], F32); gS = sb(s2b, "gS", [128, 512], F32)
            m1 = sb(s2b, "m1", [128, 512], F32); m2 = sb(s2b, "m2", [128, 512], F32)
            mergedT = sb(s2b, "mergedT", [128, 8, 512], BF16)
            h_t = Rot("h_t", [sb(s2b, "h_t%d" % i, [128, D], F32) for i in range(2)])
            stats = sb(s2b, "stats", [128, 2, 6], F32); mv = sb(s2b, "mv", [128, 2], F32); rstd = sb(s2b, "rstd", [128, 1], F32)
            for G in range(9):
                smp = (G == 8)
                ntok = NS if smp else 512
                tiles = [(0, NS)] if smp else [(ti, 128) for ti in range(4)]
                col0 = S if smp else 512 * G
                for ti, rows in tiles:
                    src = x_s if smp else x_p[512 * G + 128 * ti:512 * G + 128 * ti + 128, :]
                    DMA("sp", x_f4[:rows, ti, :], src, ["x_f4_%d" % ti], ["x_f4_%d" % ti])
                    xb, xb_r = x_b2.next()
                    CP("act", xb[:rows, :], x_f4[:rows, ti, :], ["x_f4_%d" % ti], [xb_r])
                    for kc in range(8):
                        TR(p_xT[:, kc, 0:rows], xb[:rows, kc * 128:(kc + 1) * 128], ident_b[0:rows, 0:rows], [xb_r, "ident_b"], ["p_xT2"])
                    CP("dve", xT_g[:, :, 128 * ti:128 * ti + rows], p_xT[:, :, 0:rows], ["p_xT2"], ["xT_g"])
                for j in range(2):
                    DMA("sp", s2l[:, j, 0:ntok], s2_scr[j, :, col0:col0 + ntok], ["s2l"], ["s2l"])
                    DMA("sp", oTl[:, j, 0:ntok], o_scr[j, :, col0:col0 + ntok], ["oTl"], ["oTl"])
                for dc in range(8):
                    dsl = slice(128 * dc, 128 * dc + 128)
                    for j in range(2):
                        MM(pA[:, 0:ntok], w_attn_b[:, j, dsl], oTl[:, j, 0:ntok], j == 0, j == 1, ["w_attn_b", "oTl"], ["pA"])
                    for j in range(2):
                        MM(pS_[:, 0:ntok], w_ssmbr_b[:, j, dsl], s2l[:, j, 0:ntok], j == 0, j == 1, ["w_ssmbr_b", "s2l"], ["pS_"])
                    for kc in range(8):
                        MM(pGA[:, 0:ntok], w_gate_b[:, kc, dsl], xT_g[:, kc, 0:ntok], kc == 0, kc == 7, ["w_gate_b", "xT_g"], ["pGA"])
                    for kc in range(8):
                        MM(pGS[:, 0:ntok], w_gate_b[:, kc, 1024 + 128 * dc:1024 + 128 * dc + 128], xT_g[:, kc, 0:ntok], kc == 0, kc == 7,
                           ["w_gate_b", "xT_g"], ["pGS"])
                    ACT(gA[:, 0:ntok], pGA[:, 0:ntok], AF.Sigmoid, ["pGA"], ["gA"], bias=vecs[:, 4 + dc:5 + dc])
                    ACT(gS[:, 0:ntok], pGS[:, 0:ntok], AF.Sigmoid, ["pGS"], ["gS"], bias=vecs[:, 12 + dc:13 + dc])
                    TT("dve", m1[:, 0:ntok], gA[:, 0:ntok], pA[:, 0:ntok], ALU.mult, ["gA", "pA"], ["m1"])
                    TT("dve", m2[:, 0:ntok], gS[:, 0:ntok], pS_[:, 0:ntok], ALU.mult, ["gS", "pS_"], ["m2"])
                    TT("pool", mergedT[:, dc, 0:ntok], m1[:, 0:ntok], m2[:, 0:ntok], ALU.add, ["m1", "m2"], ["mergedT"])
                for ti, rows in tiles:
                    for half in range(2):
                        for kc in range(8):
                            MM(pM[:rows, half, :], mergedT[:, kc, 128 * ti:128 * ti + rows], w_out_b[:, kc, 512 * half:512 * half + 512],
                               kc == 0, kc == 7, ["mergedT", "w_out_b"], ["pM"])
                    ht, ht_r = h_t.next()
                    for half in range(2):
                        hs = slice(512 * half, 512 * half + 512)
                        STT("dve", ht[:rows, hs], x_f4[:rows, ti, hs], ALPHA, pM[:rows, half, :], ALU.mult, ALU.add,
                            ["x_f4_%d" % ti, "pM", ht_r], [ht_r])
                    layer_norm_rows(rows, ht, ln1g_t, ln1b_t, stats, mv, rstd, ht_r)
                    r0 = col0 + 128 * ti
                    DMA("sp", x1_scr[r0:r0 + rows, :], ht[:rows, :], [ht_r], ["x1_scr"])
            if debug_stop == 2.5:
                dbg_x1 = dout("dbg_x1", [NTOK, D])
                DMA("sp", dbg_x1, x1_scr, ["x1_scr"], ["dbg_x1"])
            P.barrier()
            P.flush()
        if debug_stop == 2.5:
            return nc, {}

        with ExitStack() as s3:
            w_router_b = sb(s3, "w_router_b", [128, 8, NEXP], BF16)
            ws1_b = sb(s3, "ws1_b", [128, 8, 256], BF16); ws3_b = sb(s3, "ws3_b", [128, 8, 256], BF16)
            ws2_b = sb(s3, "ws2_b", [128, 2, D], BF16)
            w_pleg_b = sb(s3, "w_pleg_b", [128, 8, D], BF16); w_ple_b = sb(s3, "w_ple_b", [128, 2, D], BF16)
            DMA("pool", w_router_b[:], w_router.rearrange("(kc p) n -> p kc n", p=128), (), ["w_router_b"])
            DMA("pool", ws1_b[:], ws1.rearrange("(kc p) n -> p kc n", p=128), (), ["ws1_b"])
            DMA("pool", ws3_b[:], ws3.rearrange("(kc p) n -> p kc n", p=128), (), ["ws3_b"])
            DMA("pool", ws2_b[:], ws2.rearrange("(kc p) n -> p kc n", p=128), (), ["ws2_b"])
            wpv = w_ple_gate.rearrange("(kc p) n -> p kc n", p=128)
            for j in range(2):
                DMA("pool", w_pleg_b[:, :, 512 * j:512 * j + 512], wpv[:, :, 512 * j:512 * j + 512], (), ["w_pleg_b"])
            DMA("pool", w_ple_b[:], w_ple.rearrange("(kc p) n -> p kc n", p=128), (), ["w_ple_b"])
            cf = sb(s3, "cf", [128, 128], F32)
            utri_b = sb(s3, "utri_b", [128, 128], BF16); ones_b = sb(s3, "ones_b", [128, 128], BF16)
            iota_t = sb(s3, "iota_t", [128, NEXP], F32); base_t = sb(s3, "base_t", [128, NEXP], F32)
            DMA("sp", cf[:], c_utri, (), ["cf"])
            CP("dve", utri_b[:], cf[:], ["cf"], ["utri_b"])
            MSET("dve", ones_b[:], 1.0, ["ones_b"])
            DMA("sp", iota_t[:], c_iota, (), ["iota_t"])
            MSET("dve", base_t[:], 0.0, ["base_t"])
            p_T = psum(s3, "p_T3", [128, 8, 128], BF16)
            p_pT = psum(s3, "p_pT", [128, 8, 128], BF16)
            p_R = psum(s3, "p_R", [128, 512], F32)
            p_H = psum(s3, "p_H", [128, 4, 128], F32)
            p_A = psum(s3, "p_A", [128, 2, 512], F32)
            p_B = psum(s3, "p_B", [128, 2, 512], F32)
            x1f = Rot("x1f", [sb(s3, "x1f%d" % i, [128, D], F32) for i in range(2)])
            x1b = Rot("x1b", [sb(s3, "x1b%d" % i, [128, D], BF16) for i in range(2)])
            x1T = Rot("x1T", [sb(s3, "x1T%d" % i, [128, 8, 128], BF16) for i in range(2)])
            pf = Rot("pf", [sb(s3, "pf%d" % i, [128, 256], F32) for i in range(2)])
            pb = sb(s3, "pb", [128, 256], BF16); pT = sb(s3, "pT", [128, 2, 128], BF16)
            sc = sb(s3, "sc", [128, NEXP], F32); bia = sb(s3, "bia", [128, NEXP], F32)
            top8 = sb(s3, "top8", [128, 8], F32); idx8 = sb(s3, "idx8", [128, 8], U32); e8f = sb(s3, "e8f", [128, 8], F32)
            mask = sb(s3, "mask", [128, NEXP], F32); mask_b = sb(s3, "mask_b", [128, NEXP], BF16)
            ssel = sb(s3, "ssel", [128, NEXP], F32); den = sb(s3, "den", [128, 1], F32); gd = sb(s3, "gd", [128, NEXP], F32)
            rankp = sb(s3, "rankp", [128, NEXP], F32); junk = sb(s3, "junk", [128, NEXP], F32)
            rank8 = sb(s3, "rank8", [128, 8], F32); gate8 = sb(s3, "gate8", [128, 8], F32)
            destf = sb(s3, "destf", [128, 8], F32); ov = sb(s3, "ov", [128, 8], F32)
            hsf = sb(s3, "hsf", [128, 2, 128], F32); hsT = sb(s3, "hsT", [128, 2, 128], BF16)
            sg = sb(s3, "sg", [128, D], F32)
            Rt = Rot("Rt", [sb(s3, "Rt%d" % i, [128, D], F32) for i in range(2)])
            bc3 = {}
            P.ops["pool"].append(lambda e: bc3.__setitem__("r", e.to_reg(NEXP * CAP - 1)))
            for i in range(NT + 1):
                smp = (i == NT)
                rows = NS if smp else 128
                r0 = S if smp else 128 * i
                xf, xf_r = x1f.next(); xb, xb_r = x1b.next(); xT, xT_r = x1T.next(); pf_, pf_r = pf.next()
                DMA("sp", xf[:rows, :], x1_scr[r0:r0 + rows, :], ["x1_scr", xf_r], [xf_r])
                DMA("sp", pf_[:rows, :], (p_s if smp else p_p[r0:r0 + rows, :]), [pf_r], [pf_r])
                CP("act", xb[:rows, :], xf[:rows, :], [xf_r], [xb_r])
                for kc in range(8):
                    TR(p_T[:, kc, 0:rows], xb[:rows, kc * 128:(kc + 1) * 128], ident_b[0:rows, 0:rows], [xb_r, "ident_b"], ["p_T3"])
                CP("dve", xT[:, :, 0:rows], p_T[:, :, 0:rows], ["p_T3"], [xT_r])
                CP("act", pb[:rows, :], pf_[:rows, :], [pf_r], ["pb"])
                for j in range(2):
                    TR(p_pT[:, j, 0:rows], pb[:rows, j * 128:(j + 1) * 128], ident_b[0:rows, 0:rows], ["pb", "ident_b"], ["p_pT"])
                CP("dve", pT[:, :, 0:rows], p_pT[:, 0:2, 0:rows], ["p_pT"], ["pT"])
                for kc in range(8):
                    MM(p_R[:rows, 0:64], xT[:, kc, 0:rows], w_router_b[:, kc, :], kc == 0, kc == 7, [xT_r, "w_router_b"], ["p_R"])
                ACT(sc[:rows, :], p_R[:rows, 0:64], AF.Sigmoid, ["p_R"], ["sc"])
                TT("dve", bia[:rows, :], sc[:rows, :], rbias_t[:rows, :], ALU.add, ["sc", "rbias"], ["bia"])
                P.op("dve", lambda e, rows=rows: e.max(out=top8[:rows, :], in_=bia[:rows, :]), ["bia"], ["top8"])
                P.op("dve", lambda e, rows=rows: e.max_index(out=idx8[:rows, :], in_max=top8[:rows, :], in_values=bia[:rows, :]),
                     ["bia", "top8"], ["idx8"])
                CP("dve", e8f[:rows, :], idx8[:rows, :], ["idx8"], ["e8f"])
                TS("dve", mask[:rows, :], bia[:rows, :], top8[:rows, 7:8], None, ALU.is_ge, None, ["bia", "top8"], ["mask"])
                CP("dve", mask_b[:rows, :], mask[:rows, :], ["mask"], ["mask_b"])
                TT("dve", ssel[:rows, :], sc[:rows, :], mask[:rows, :], ALU.mult, ["sc", "mask"], ["ssel"])
                P.op("dve", lambda e, rows=rows: e.tensor_reduce(out=den[:rows, :], in_=ssel[:rows, :], axis=AX.X, op=ALU.add), ["ssel"], ["den"])
                P.op("dve", lambda e, rows=rows: e.reciprocal(out=den[:rows, :], in_=den[:rows, :]), ["den"], ["den"])
                TS("dve", gd[:rows, :], ssel[:rows, :], den[:rows, 0:1], 2.5, ALU.mult, ALU.mult, ["ssel", "den"], ["gd"])
                MM(p_R[:rows, 64:128], utri_b[:rows, :rows], mask_b[:rows, :], True, True, ["utri_b", "mask_b"], ["p_R"])
                TT("dve", rankp[:rows, :], p_R[:rows, 64:128], base_t[:rows, :], ALU.add, ["p_R", "base_t"], ["rankp"])
                if not smp:
                    MM(p_R[:, 128:192], ones_b[:, :], mask_b[:, :], True, True, ["ones_b", "mask_b"], ["p_R"])
                    TT("dve", base_t[:], base_t[:], p_R[:, 128:192], ALU.add, ["p_R", "base_t"], ["base_t"])
                for k in range(8):
                    P.op("dve", lambda e, rows=rows, k=k: e.scalar_tensor_tensor(
                        out=junk[:rows, :], in0=iota_t[:rows, :], scalar=e8f[:rows, k:k + 1], in1=rankp[:rows, :],
                        op0=ALU.is_equal, op1=ALU.mult, accum_out=rank8[:rows, k:k + 1]), ["iota_t", "e8f", "rankp", "junk"], ["junk", "rank8"])
                    P.op("dve", lambda e, rows=rows, k=k: e.scalar_tensor_tensor(
                        out=junk[:rows, :], in0=iota_t[:rows, :], scalar=e8f[:rows, k:k + 1], in1=gd[:rows, :],
                        op0=ALU.is_equal, op1=ALU.mult, accum_out=gate8[:rows, k:k + 1]), ["iota_t", "e8f", "gd", "junk"], ["junk", "gate8"])
                STT("dve", destf[:rows, :], e8f[:rows, :], float(CAP), rank8[:rows, :], ALU.mult, ALU.add, ["e8f", "rank8"], ["destf"])
                P.op("dve", lambda e, rows=rows: e.tensor_single_scalar(out=ov[:rows, :], in_=rank8[:rows, :], scalar=float(CAP) - 0.5, op=ALU.is_gt),
                     ["rank8"], ["ov"])
                STT("dve", destf[:rows, :], ov[:rows, :], 1.0e6, destf[:rows, :], ALU.mult, ALU.add, ["ov", "destf"], ["destf"])
                TS("dve", ov[:rows, :], ov[:rows, :], -1.0, 1.0, ALU.mult, ALU.add, ["ov"], ["ov"])
                TT("dve", gate_t[:rows, i, :], gate8[:rows, :], ov[:rows, :], ALU.mult, ["gate8", "ov"], ["gate_t"])
                CP("dve", dest_t[:rows, i, :], destf[:rows, :], ["destf"], ["dest_t"])
                for k in range(8):
                    P.dma("pool", lambda e, rows=rows, i=i, k=k, xb=xb: e.indirect_dma_start(
                        out=xg_scr, out_offset=bass.IndirectOffsetOnAxis(ap=dest_t[:, i, k:k + 1], axis=0),
                        in_=xb[:, :], in_offset=None, bounds_check=bc3["r"], oob_is_err=False),
                        [xb_r, "dest_t"], ["xg_scr"])
                for a_, wsx in enumerate((ws1_b, ws3_b)):
                    for ffc in range(2):
                        for kc in range(8):
                            MM(p_H[:, 2 * a_ + ffc, 0:rows], wsx[:, kc, 128 * ffc:128 * ffc + 128], xT[:, kc, 0:rows], kc == 0, kc == 7,
                               ["ws1_b", "ws3_b", xT_r], ["p_H"])
                ACT(hsf[:, :, 0:rows], p_H[:, 0:2, 0:rows], AF.Silu, ["p_H"], ["hsf"])
                TT("dve", hsT[:, :, 0:rows], hsf[:, :, 0:rows], p_H[:, 2:4, 0:rows], ALU.mult, ["hsf", "p_H"], ["hsT"])
                for half in range(2):
                    hs = slice(512 * half, 512 * half + 512)
                    for ffc in range(2):
                        MM(p_A[:rows, half, :], hsT[:, ffc, 0:rows], ws2_b[:, ffc, hs], ffc == 0, ffc == 1, ["hsT", "ws2_b"], ["p_A"])
                    for kc in range(8):
                        MM(p_B[:rows, half, :], xT[:, kc, 0:rows], w_pleg_b[:, kc, hs], kc == 0, kc == 7, [xT_r, "w_pleg_b"], ["p_B"])
                    ACT(sg[:rows, hs], p_B[:rows, half, :], AF.Sigmoid, ["p_B"], ["sg"])
                Rt_, Rt_r = Rt.next()
                for half in range(2):
                    hs = slice(512 * half, 512 * half + 512)
                    for pc in range(2):
                        MM(p_B[:rows, half, :], pT[:, pc, 0:rows], w_ple_b[:, pc, hs], pc == 0, pc == 1, ["pT", "w_ple_b", "sg"], ["p_B"])
                    STT("dve", Rt_[:rows, hs], xf[:rows, hs], ALPHA, p_A[:rows, half, :], ALU.mult, ALU.add, [xf_r, "p_A", Rt_r], [Rt_r])
                    TT("dve", sg[:rows, hs], sg[:rows, hs], p_B[:rows, half, :], ALU.mult, ["sg", "p_B"], ["sg"])
                    TT("pool", Rt_[:rows, hs], Rt_[:rows, hs], sg[:rows, hs], ALU.add, [Rt_r, "sg"], [Rt_r])
                DMA("sp", r_scr[r0:r0 + rows, :], Rt_[:rows, :], [Rt_r], ["r_scr"])
            if debug_stop == 3:
                dbg_r = dout("dbg_r", [NTOK, D])
                dbg_gate = dout("dbg_gate", [128, (NT + 1) * 8])
                dbg_dest = dout("dbg_dest", [128, (NT + 1) * 8], I32)
                DMA("sp", dbg_r, r_scr, ["r_scr"], ["dbg_r"])
                DMA("sp", dbg_gate, gate_t[:].rearrange("p a b -> p (a b)"), ["gate_t"], ["dbg_gate"])
                DMA("sp", dbg_dest, dest_t[:].rearrange("p a b -> p (a b)"), ["dest_t"], ["dbg_dest"])
            P.barrier()
            P.flush()
        if debug_stop == 3:
            return nc, {}

        with ExitStack() as s4:
            w1b = Rot("w1b", [sb(s4, "w1b%d" % i, [128, 8, 256], BF16) for i in range(2)])
            w3b = Rot("w3b", [sb(s4, "w3b%d" % i, [128, 8, 256], BF16) for i in range(2)])
            w2b = Rot("w2b", [sb(s4, "w2b%d" % i, [128, 2, D], BF16) for i in range(2)])
            xg = Rot("xg", [sb(s4, "xg%d" % i, [128, 4, D], BF16) for i in range(2)])
            xgT = sb(s4, "xgT", [128, 8, 512], BF16)
            hf = sb(s4, "hf", [128, 2, 512], F32)
            hT = sb(s4, "hT", [128, 2, 512], BF16)
            yb = Rot("yb", [sb(s4, "yb%d" % i, [128, D], BF16) for i in range(3)])
            pTa = psum(s4, "pTa", [128, 4, 512], BF16)
            pH = psum(s4, "pH", [128, 4, 512], F32)
            pY = psum(s4, "pY", [128, 2, 512], F32)
            NEX = NEXP if (debug_stop is None or debug_stop >= 4) else 1
            xgT2 = [xgT, sb(s4, "xgT_b", [128, 8, 512], BF16)]
            NSB = CAP // 512
            sbs = [(ex, sbk) for ex in range(NEX) for sbk in range(NSB)]
            wcur = {}

            def stage_T(idx):
                ex, sbk = sbs[idx]
                if sbk == 0:
                    w1_, w1_r = w1b.next(); w3_, w3_r = w3b.next(); w2_, w2_r = w2b.next()
                    DMA("pool", w1_[:], w1[ex].rearrange("(kc p) n -> p kc n", p=128), [w1_r], [w1_r])
                    DMA("pool", w3_[:], w3[ex].rearrange("(kc p) n -> p kc n", p=128), [w3_r], [w3_r])
                    DMA("pool", w2_[:], w2[ex].rearrange("(kc p) n -> p kc n", p=128), [w2_r], [w2_r])
                    wcur[ex] = (w1_, w1_r, w3_, w3_r, w2_, w2_r)
                row0 = ex * CAP + 512 * sbk
                xg_, xg_r = xg.next()
                DMA("sp", xg_[:], xg_scr[row0:row0 + 512, :].rearrange("(b p) d -> p b d", p=128), ["xg_scr", xg_r], [xg_r])
                xT_ = xgT2[idx % 2]
                for half in range(2):
                    for kcl in range(4):
                        kc = 4 * half + kcl
                        for blk in range(4):
                            TR(pTa[:, kcl, 128 * blk:128 * blk + 128], xg_[:, blk, 128 * kc:128 * kc + 128], ident_b[:],
                               [xg_r, "ident_b"], ["pTa"])
                    for kcl in range(4):
                        CP("dve", xT_[:, 4 * half + kcl, :], pTa[:, kcl, :], ["pTa"], ["xgT%d_%d" % (idx % 2, 4 * half + kcl)])

            def stage_H(idx):
                ex, sbk = sbs[idx]
                w1_, w1_r, w3_, w3_r, w2_, w2_r = wcur[ex]
                xT_ = xgT2[idx % 2]
                for a_, wx, wx_r in ((0, w1_, w1_r), (1, w3_, w3_r)):
                    for ffc in range(2):
                        for kc in range(8):
                            MM(pH[:, 2 * a_ + ffc, :], wx[:, kc, 128 * ffc:128 * ffc + 128], xT_[:, kc, :], kc == 0, kc == 7,
                               [wx_r, "xgT%d_%d" % (idx % 2, kc)], ["pH%d" % (2 * a_ + ffc)])
                for ffc in range(2):
                    ACT(hf[:, ffc, :], pH[:, ffc, :], AF.Silu, ["pH%d" % ffc], ["hf%d" % ffc])
                    TT("dve", hT[:, ffc, :], hf[:, ffc, :], pH[:, 2 + ffc, :], ALU.mult, ["hf%d" % ffc, "pH%d" % (2 + ffc)], ["hT%d" % ffc])

            def stage_Y(idx):
                ex, sbk = sbs[idx]
                w1_, w1_r, w3_, w3_r, w2_, w2_r = wcur[ex]
                row0 = ex * CAP + 512 * sbk
                for blk in range(4):
                    for half in range(2):
                        for ffc in range(2):
                            MM(pY[:, half, :], hT[:, ffc, 128 * blk:128 * blk + 128], w2_[:, ffc, 512 * half:512 * half + 512],
                               ffc == 0, ffc == 1, ["hT%d" % ffc, w2_r], ["pY%d" % half])
                    yb_, yb_r = yb.next()
                    CP("act", yb_[:, 0:512], pY[:, 0, :], ["pY0", yb_r], [yb_r])
                    CP("dve", yb_[:, 512:1024], pY[:, 1, :], ["pY1", yb_r], [yb_r])
                    DMA("sp", yg_scr[row0 + 128 * blk:row0 + 128 * blk + 128, :], yb_[:], [yb_r], ["yg_scr"])

            stage_T(0)
            for idx in range(len(sbs)):
                stage_H(idx)
                if idx + 1 < len(sbs):
                    stage_T(idx + 1)
                stage_Y(idx)
            P.barrier()
            P.flush()

        with ExitStack() as s5:
            ln2g_t = sb(s5, "ln2g_t", [128, D], F32); ln2b_t = sb(s5, "ln2b_t", [128, D], F32)
            DMA("sp", ln2g_t[:], ln2_g.partition_broadcast(128), (), ["ln_g"])
            DMA("sp", ln2b_t[:], ln2_b.partition_broadcast(128), (), ["ln_b"])
            Gb = [sb(s5, "Gb%d" % k, [128, D], BF16) for k in range(8)]
            for k in range(8):
                MSET("pool" if k % 2 else "dve", Gb[k][:], 0.0, ["Gb%d" % k])
            Rt5 = Rot("Rt5", [sb(s5, "Rt5_%d" % i, [128, D], F32) for i in range(2)])
            stats = sb(s5, "stats5", [128, 2, 6], F32); mv = sb(s5, "mv5", [128, 2], F32); rstd = sb(s5, "rstd5", [128, 1], F32)
            bc5 = {}
            P.ops["pool"].append(lambda e: bc5.__setitem__("r", e.to_reg(NEXP * CAP - 1)))
            for i in range(NT + 1):
                smp = (i == NT)
                rows = NS if smp else 128
                r0 = S if smp else 128 * i
                Rt_, Rt_r = Rt5.next()
                DMA("sp", Rt_[:rows, :], r_scr[r0:r0 + rows, :], ["r_scr", Rt_r], [Rt_r])
                for k in range(8):
                    P.dma("pool", lambda e, rows=rows, i=i, k=k: e.indirect_dma_start(
                        out=Gb[k][:, :], out_offset=None, in_=yg_scr,
                        in_offset=bass.IndirectOffsetOnAxis(ap=dest_t[:, i, k:k + 1], axis=0),
                        bounds_check=bc5["r"], oob_is_err=False), ["yg_scr", "dest_t", "Gb%d" % k], ["Gb%d" % k])
                for k in range(8):
                    STT("dve", Rt_[:rows, :], Gb[k][:rows, :], gate_t[:rows, i, k:k + 1], Rt_[:rows, :], ALU.mult, ALU.add,
                        ["Gb%d" % k, "gate_t", Rt_r], [Rt_r])
                layer_norm_rows(rows, Rt_, ln2g_t, ln2b_t, stats, mv, rstd, Rt_r)
                DMA("sp", (y_s if smp else y_p[r0:r0 + rows, :]), Rt_[:rows, :], [Rt_r], ["y_out"])
            P.barrier()
            P.flush()
    return nc, {}


_NC_CACHE = {}


def _consts():
    half = 32
    inv = (np.float32(10000.0) ** (-np.arange(half, dtype=np.float32) / np.float32(half))).astype(np.float32)
    rope = np.zeros((3, S, 64), np.float32)
    for g, dil in enumerate(DILS):
        L = S // dil
        idx = np.arange(S)
        r = idx // L
        j = idx % L
        pos = (j * dil + r).astype(np.float32)
        ang = pos[:, None] * inv[None, :]
        rope[g, :, :32] = np.cos(ang)
        rope[g, :, 32:] = np.sin(ang)
    ang_s = np.float32(8192.0) * inv
    rope_s = np.concatenate([np.cos(ang_s), np.sin(ang_s)])[None, :].astype(np.float32)
    kk = np.arange(128)[:, None]
    qq = np.arange(128)[None, :]
    am = np.zeros((128, 4, 2, 128), np.float32)
    am[:, :, 0, :] = (kk >= qq)[:, None, :]
    am[:, :, 1, :] = (kk <= qq)[:, None, :]
    sel = (np.arange(128)[None, :] // 8 == np.arange(16)[:, None]).astype(np.float32)
    return {
        "c_ident": np.eye(128, dtype=np.float32),
        "c_rope": rope,
        "c_rope_s": rope_s,
        "c_amask": am.reshape(128, 1024),
        "c_utri": (kk < qq).astype(np.float32),
        "c_iota": np.tile(np.arange(64, dtype=np.float32)[None, :], (128, 1)),
        "c_sel": sel,
        "c_selT": np.ascontiguousarray(sel.T),
    }


def make_in_maps(inp, ne_in=NEXP):
    c = _consts()
    f = lambda a: np.ascontiguousarray(a, dtype=np.float32)
    shared = {
        "w_in": f(inp["w_in"][0]), "a_re": f(inp["a_re"][0]), "a_im": f(inp["a_im"][0]), "log_dt": f(inp["log_dt"][0]),
        "b_re": f(inp["b_re"][0]), "b_im": f(inp["b_im"][0]), "c_re": f(inp["c_re"][0]), "c_im": f(inp["c_im"][0]),
        "d_skip": f(inp["d_skip"][0]), "w_glu": f(inp["w_glu"][0]), "b_glu": f(inp["b_glu"][0]),
        "w_attn_br": f(inp["w_attn_br"][0]), "w_ssm_br": f(inp["w_ssm_br"][0]), "w_gate": f(inp["w_gate"][0]),
        "b_gate": f(inp["b_gate"][0]), "w_out": f(inp["w_out"][0]), "ln1_g": f(inp["ln1_g"][0]), "ln1_b": f(inp["ln1_b"][0]),
        "w_router": f(inp["w_router"][0]), "router_bias": f(inp["router_bias"][0]),
        "w1": f(inp["w1"][0, :ne_in]), "w3": f(inp["w3"][0, :ne_in]), "w2": f(inp["w2"][0, :ne_in]),
        "ws1": f(inp["ws1"][0]), "ws3": f(inp["ws3"][0]), "ws2": f(inp["ws2"][0]),
        "w_ple_gate": f(inp["w_ple_gate"][0]), "w_ple": f(inp["w_ple"][0]),
        "ln2_g": f(inp["ln2_g"][0]), "ln2_b": f(inp["ln2_b"][0]),
    }
    shared.update(c)
    maps = []
    for i in range(8):
        sl = slice(NS * i, NS * i + NS)
        m = dict(shared)
        m["x_p"] = f(inp["x_prompt"][i])
        m["x_s"] = f(inp["x_sample"][sl, 0])
        m["cache0"] = f(inp["cache_kv_w128"][0, sl]).reshape(NS, 128, 512)
        m["cache1"] = f(inp["cache_kv_w512"][0, sl]).reshape(NS, 512, 512)
        m["cache2"] = f(inp["cache_kv_w2048"][0, sl]).reshape(NS, 2048, 512)
        m["st_ssm"] = f(inp["state_ssm"][0, sl]).reshape(NS, 2048)
        m["p_p"] = f(inp["p_prompt"][0, i])
        m["p_s"] = f(inp["p_sample"][0, sl, 0])
        maps.append(m)
    return maps


def kernel(**inp):
    if "nc" not in _NC_CACHE:
        _NC_CACHE["nc"] = build()[0]
    nc = _NC_CACHE["nc"]
    maps = make_in_maps(inp)
    res = run_bass_kernel_spmd(nc, maps, core_ids=list(range(8))).results
    cat = lambda k: np.stack([r[k] for r in res], 0)
    y_p = cat("y_p").reshape(8, S, D)
    y_s = np.concatenate([r["y_s"] for r in res], 0).reshape(128, 1, D)
    outs = [y_p, y_s]
    for g in range(3):
        outs.append(cat("kv_p%d" % g).reshape(1, 8, KEEP[g], 2, 4, 64))
        outs.append(np.concatenate([r["kv_s%d" % g] for r in res], 0).reshape(1, 128, 1, 2, 4, 64))
    outs.append(cat("ssm_p").reshape(1, 8, 16, 64, 2))
    outs.append(np.concatenate([r["ssm_s"] for r in res], 0).reshape(1, 128, 16, 64, 2))
    return tuple(np.ascontiguousarray(o, dtype=np.float32) for o in outs)
```
